# Optimizing a Trainium2 kernel written in Bass

```python
import jax, jax.numpy as jnp
from jax import lax
import numpy as np

D_MODEL = 2048
BATCH = 8
SEQ = 2048
DEPTH = 1

HEAD_DIM = 128
N_HEADS_A = 8
N_HEADS_B = 8
D_A = N_HEADS_A * HEAD_DIM
D_B = N_HEADS_B * HEAD_DIM
D_MIX = D_A + D_B
IDX_HEADS = 16
IDX_DIM = 64
DSA_TOPK_MAX = 256
MOBA_BLOCK = 256
MOBA_TOPK_MAX = 3
DSA_Q_CHUNK = 64
MOBA_Q_CHUNK = 16
RMS_EPS = 1e-6
NEG_INF = -1e30

COL_WIDTHS = [D_A, D_A, D_A, D_A,
              IDX_HEADS * IDX_DIM, IDX_DIM, IDX_HEADS,
              D_B, D_B, D_B, D_B]
D_IN = sum(COL_WIDTHS)
COL_SPLITS = [int(s) for s in np.cumsum(COL_WIDTHS)[:-1]]

kernel_name = "hybrid_dsa_moba_adaln_block"


def _rms_norm(x, g):
    xf = x.astype(jnp.float32)
    y = xf * lax.rsqrt(jnp.mean(xf * xf, axis=-1, keepdims=True) + RMS_EPS)
    return (y * g.astype(jnp.float32)).astype(x.dtype)


def _alibi_slopes(n):
    return jnp.asarray(2.0 ** (-8.0 * np.arange(1, n + 1) / n), dtype=jnp.float32)


def _dsa_attention(q, k, v, q_idx, k_idx, w_idx, slopes):
    B, L, H, Dh = q.shape
    top_k = min(DSA_TOPK_MAX, L // 4)
    n_chunks = L // DSA_Q_CHUNK
    key_pos = jnp.arange(L)
    gather = jax.vmap(lambda t, i: t[i])

    def to_chunks(t):
        return jnp.swapaxes(t.reshape((B, n_chunks, DSA_Q_CHUNK) + t.shape[2:]), 0, 1)

    def chunk_fn(args):
        start, qc, qic, wc = args
        q_pos = start + jnp.arange(DSA_Q_CHUNK)
        logits = jnp.einsum('bqhd,bsd->bqhs', qic, k_idx).astype(jnp.float32) * IDX_DIM ** -0.5
        score = jnp.einsum('bqh,bqhs->bqs', wc.astype(jnp.float32), jax.nn.relu(logits)) * IDX_HEADS ** -0.5
        causal = key_pos[None, :] <= q_pos[:, None]
        score = jnp.where(causal[None], score, NEG_INF)
        _, sel = lax.top_k(score, top_k)
        valid = sel <= q_pos[None, :, None]
        k_sel = gather(k, sel)
        v_sel = gather(v, sel)
        s = jnp.einsum('bqhd,bqkhd->bqhk', qc, k_sel).astype(jnp.float32) * Dh ** -0.5
        dist = (q_pos[None, :, None] - sel).astype(jnp.float32)
        s = s - slopes[None, None, :, None] * dist[:, :, None, :]
        s = jnp.where(valid[:, :, None, :], s, NEG_INF)
        p = jax.nn.softmax(s, axis=-1).astype(v.dtype)
        return jnp.einsum('bqhk,bqkhd->bqhd', p, v_sel)

    starts = jnp.arange(n_chunks) * DSA_Q_CHUNK
    out = lax.map(chunk_fn, (starts, to_chunks(q), to_chunks(q_idx), to_chunks(w_idx)))
    return jnp.swapaxes(out, 0, 1).reshape(B, L, H, Dh)


def _moba_attention(q, k, v, slopes):
    B, L, H, Dh = q.shape
    nb = -(-L // MOBA_BLOCK)
    Lp = nb * MOBA_BLOCK
    pad = ((0, 0), (0, Lp - L), (0, 0), (0, 0))
    kp = jnp.pad(k, pad)
    vp = jnp.pad(v, pad)
    qp = jnp.pad(q, pad)
    scale = Dh ** -0.5
    offs = jnp.arange(MOBA_BLOCK)
    qb = qp.reshape(B, nb, MOBA_BLOCK, H, Dh)
    kb = kp.reshape(B, nb, MOBA_BLOCK, H, Dh)
    vb = vp.reshape(B, nb, MOBA_BLOCK, H, Dh)

    s_self = jnp.einsum('bnqhd,bnshd->bnhqs', qb, kb).astype(jnp.float32) * scale
    rel = offs[:, None] - offs[None, :]
    s_self = s_self - slopes[:, None, None] * rel.astype(jnp.float32)
    s_self = jnp.where(rel >= 0, s_self, NEG_INF)
    lse_self = jax.nn.logsumexp(s_self, axis=-1)
    p_self = jnp.exp(s_self - lse_self[..., None]).astype(v.dtype)
    o_self = jnp.einsum('bnhqs,bnshd->bnqhd', p_self, vb).reshape(B, Lp, H, Dh)[:, :L]
    lse_self = jnp.swapaxes(lse_self, 2, 3).reshape(B, Lp, H)[:, :L]

    top_k = min(MOBA_TOPK_MAX, nb - 1)
    if top_k == 0:
        return o_self.astype(q.dtype)

    k_mean = jnp.mean(kb.astype(jnp.float32), axis=2)
    gate = jnp.einsum('bthd,bnhd->bthn', q.astype(jnp.float32), k_mean)
    q_blk = jnp.arange(L) // MOBA_BLOCK
    past = jnp.arange(nb)[None, :] < q_blk[:, None]
    gate = jnp.where(past[None, :, None, :], gate, NEG_INF)
    _, sel = lax.top_k(gate, top_k)

    k_bh = jnp.transpose(kb, (0, 3, 1, 2, 4))
    v_bh = jnp.transpose(vb, (0, 3, 1, 2, 4))
    bi = jnp.arange(B)[:, None, None, None]
    hi = jnp.arange(H)[None, None, :, None]
    n_chunks = L // MOBA_Q_CHUNK

    def to_chunks(t):
        return jnp.swapaxes(t.reshape((B, n_chunks, MOBA_Q_CHUNK) + t.shape[2:]), 0, 1)

    def chunk_fn(args):
        start, qc, sc = args
        q_pos = start + jnp.arange(MOBA_Q_CHUNK)
        valid = sc < (q_pos // MOBA_BLOCK)[None, :, None, None]
        k_g = k_bh[bi, hi, sc]
        v_g = v_bh[bi, hi, sc]
        s = jnp.einsum('bqhd,bqhjsd->bqhjs', qc, k_g).astype(jnp.float32) * scale
        key_pos = sc[..., None] * MOBA_BLOCK + offs
        dist = (q_pos[None, :, None, None, None] - key_pos).astype(jnp.float32)
        s = s - slopes[None, None, :, None, None] * dist
        s = jnp.where(valid[..., None], s, NEG_INF)
        s = s.reshape(B, MOBA_Q_CHUNK, H, top_k * MOBA_BLOCK)
        lse = jax.nn.logsumexp(s, axis=-1)
        p = jnp.exp(s - lse[..., None]).astype(v.dtype).reshape(B, MOBA_Q_CHUNK, H, top_k, MOBA_BLOCK)
        o = jnp.einsum('bqhjs,bqhjsd->bqhd', p, v_g)
        return o, lse

    starts = jnp.arange(n_chunks) * MOBA_Q_CHUNK
    o_hist, lse_hist = lax.map(chunk_fn, (starts, to_chunks(q), to_chunks(sel)))
    o_hist = jnp.swapaxes(o_hist, 0, 1).reshape(B, L, H, Dh)
    lse_hist = jnp.swapaxes(lse_hist, 0, 1).reshape(B, L, H)

    lse = jnp.logaddexp(lse_self, lse_hist)
    w_self = jnp.exp(lse_self - lse)[..., None]
    w_hist = jnp.exp(lse_hist - lse)[..., None]
    return (o_self * w_self + o_hist * w_hist).astype(q.dtype)


def _hybrid_layer(x, c, w_ada, b_ada, g_norm, w_in, q_norm_a, k_norm_a, k_norm_idx,
                  q_norm_b, k_norm_b, w_out):
    B, L, _ = x.shape
    mod = jax.nn.silu(c) @ w_ada + b_ada
    shift, scale, gate = jnp.split(mod, 3, axis=-1)
    h = _rms_norm(x, g_norm) * (1.0 + scale[:, None, :]) + shift[:, None, :]

    proj = h @ w_in
    qa, ka, va, za, q_idx, k_idx, w_idx, qb, kb, vb, zb = jnp.split(proj, COL_SPLITS, axis=-1)

    heads_a = (B, L, N_HEADS_A, HEAD_DIM)
    heads_b = (B, L, N_HEADS_B, HEAD_DIM)
    qa = _rms_norm(qa.reshape(heads_a), q_norm_a)
    ka = _rms_norm(ka.reshape(heads_a), k_norm_a)
    va = va.reshape(heads_a)
    q_idx = q_idx.reshape(B, L, IDX_HEADS, IDX_DIM)
    k_idx = _rms_norm(k_idx, k_norm_idx)
    qb = _rms_norm(qb.reshape(heads_b), q_norm_b)
    kb = _rms_norm(kb.reshape(heads_b), k_norm_b)
    vb = vb.reshape(heads_b)

    ya = _dsa_attention(qa, ka, va, q_idx, k_idx, w_idx, _alibi_slopes(N_HEADS_A))
    yb = _moba_attention(qb, kb, vb, _alibi_slopes(N_HEADS_B))

    ya = ya.reshape(B, L, D_A) * jax.nn.silu(za)
    yb = yb.reshape(B, L, D_B) * jax.nn.silu(zb)
    y = jnp.concatenate([ya, yb], axis=-1) @ w_out
    return x + gate[:, None, :] * y


def setup_inputs(seed: int = 0) -> dict:
    key = jax.random.key(seed)
    ks = jax.random.split(key, 12)
    nrm = jax.random.normal
    x = nrm(ks[0], (BATCH, SEQ, D_MODEL), jnp.float32)
    c = nrm(ks[1], (BATCH, D_MODEL), jnp.float32)
    w_ada = nrm(ks[2], (DEPTH, D_MODEL, 3 * D_MODEL), jnp.float32) * (0.5 * D_MODEL ** -0.5)
    b_ada = 0.01 * nrm(ks[3], (DEPTH, 3 * D_MODEL), jnp.float32)
    g_norm = 1.0 + 0.02 * nrm(ks[4], (DEPTH, D_MODEL), jnp.float32)
    w_in = nrm(ks[5], (DEPTH, D_MODEL, D_IN), jnp.float32) * D_MODEL ** -0.5
    q_norm_a = 1.0 + 0.02 * nrm(ks[6], (DEPTH, HEAD_DIM), jnp.float32)
    k_norm_a = 1.0 + 0.02 * nrm(ks[7], (DEPTH, HEAD_DIM), jnp.float32)
    k_norm_idx = 1.0 + 0.02 * nrm(ks[8], (DEPTH, IDX_DIM), jnp.float32)
    q_norm_b = 1.0 + 0.02 * nrm(ks[9], (DEPTH, HEAD_DIM), jnp.float32)
    k_norm_b = 1.0 + 0.02 * nrm(ks[10], (DEPTH, HEAD_DIM), jnp.float32)
    w_out = nrm(ks[11], (DEPTH, D_MIX, D_MODEL), jnp.float32) * D_MIX ** -0.5
    return {"x": x, "c": c, "w_ada": w_ada, "b_ada": b_ada, "g_norm": g_norm, "w_in": w_in,
            "q_norm_a": q_norm_a, "k_norm_a": k_norm_a, "k_norm_idx": k_norm_idx,
            "q_norm_b": q_norm_b, "k_norm_b": k_norm_b, "w_out": w_out}


def reference(x, c, w_ada, b_ada, g_norm, w_in, q_norm_a, k_norm_a, k_norm_idx,
              q_norm_b, k_norm_b, w_out):
    for i in range(DEPTH):
        x = _hybrid_layer(x, c, w_ada[i], b_ada[i], g_norm[i], w_in[i], q_norm_a[i], k_norm_a[i],
                          k_norm_idx[i], q_norm_b[i], k_norm_b[i], w_out[i])
    return x
```

```python
import numpy as np
import ml_dtypes
import concourse.bass as bass
import concourse.mybir as mybir
from concourse.bass_utils import run_bass_kernel_spmd

F32 = mybir.dt.float32
BF16 = mybir.dt.bfloat16
AF = mybir.ActivationFunctionType
ALU = mybir.AluOpType
AX = mybir.AxisListType

L = 2048
D = 2048
NB = 16
DIN = 9296
NIT = 14
TOPK = 256
MASKV = -60000.0
EPS = 1e-6
NEG = -1.0e30

ENGS = ["pe", "act", "dve", "pool", "sp"]
NDMASEM = 24
SELF_SYNC = True
UNITS = list(range(16))
STOP_UI = 0


class Tok:
    __slots__ = ("eng", "idx", "needed", "value", "semkey")

    def __init__(self, eng, idx):
        self.eng = eng
        self.idx = idx
        self.needed = False
        self.value = None
        self.semkey = eng


class Sched:
    def __init__(self):
        self.ops = {e: [] for e in ENGS}
        self.res = {}
        self.dma_n = 0
        self.dma_last = {}

    def _collect(self, eng, reads, writes, waits):
        ws = list(waits)
        for k in reads:
            r = self.res.get(k)
            if r is not None and r[0] is not None:
                ws.append(r[0])
        for k in writes:
            r = self.res.get(k)
            if r is not None:
                if r[0] is not None:
                    ws.append(r[0])
                ws.extend(r[1].values())
        best = {}
        for w in ws:
            if w is None:
                continue
            if w.eng == "sp":
                best[("d", w.semkey, w.value)] = w
                continue
            if w.eng == eng and (eng == "pe" or not SELF_SYNC):
                continue
            b = best.get(w.eng)
            if b is None or w.idx > b.idx:
                best[w.eng] = w
        out = list(best.values())
        for w in out:
            w.needed = True
        return out

    def op(self, eng, fn, reads=(), writes=(), waits=()):
        if eng != "pe":
            psr = [k for k in reads if isinstance(k, tuple) and k[0] == "ps"]
            if psr:
                reads = [k for k in reads if k not in psr]
                writes = list(writes) + [k for k in psr if k not in writes]
        ws = self._collect(eng, reads, writes, waits)
        tok = Tok(eng, len(self.ops[eng]))
        for k in reads:
            r = self.res.setdefault(k, [None, {}])
            r[1][eng] = tok
        for k in writes:
            self.res[k] = [tok, {}]
        self.ops[eng].append((fn, ws, tok))
        return tok

    def dma(self, fn, reads=(), writes=(), waits=()):
        k = self.dma_n % NDMASEM
        m = self.dma_n // NDMASEM + 1
        self.dma_n += 1
        ws = list(waits)
        prev = self.dma_last.get(k)
        if prev is not None:
            ws.append(prev)
        wl = self._collect("sp", reads, writes, ws)
        tok = Tok("sp", len(self.ops["sp"]))
        tok.semkey = ("dma", k)
        tok.value = 16 * m
        tok.needed = True
        self.dma_last[k] = tok
        for kk in reads:
            r = self.res.setdefault(kk, [None, {}])
            r[1]["sp"] = tok
        for kk in writes:
            self.res[kk] = [tok, {}]
        self.ops["sp"].append((fn, wl, tok))
        return tok

    def barrier(self):
        last = {}
        for e in ENGS:
            for (fn, ws, tok) in reversed(self.ops[e]):
                if fn is not None:
                    last[e] = tok
                    break
        if "sp" in last:
            sp_toks = list(self.dma_last.values())
        else:
            sp_toks = []
        for e in ENGS:
            ws = [t for (ee, t) in last.items() if ee != e and ee != "sp"] + (sp_toks if e != "sp" else [])
            for w in ws:
                w.needed = True
            self.ops[e].append((None, ws, None))

    def emit(self, nc, block, sems, dsems):
        for e in ENGS:
            if e == "sp":
                continue
            c = 0
            for (fn, ws, tok) in self.ops[e]:
                if tok is not None and tok.needed:
                    c += 1
                    tok.value = c

        def semof(w):
            if w.eng == "sp":
                return dsems[w.semkey[1]]
            return sems[w.eng]

        def run(ename):
            def body(eng):
                waited = {}
                for (fn, ws, tok) in self.ops[ename]:
                    for w in ws:
                        key = w.semkey
                        if waited.get(key, 0) >= w.value:
                            continue
                        eng.wait_ge(semof(w), w.value)
                        waited[key] = w.value
                    if fn is None:
                        continue
                    ins = fn(eng)
                    if ename == "sp":
                        ins.then_inc(semof(tok), 16)
                    elif tok.needed:
                        ins.then_inc(sems[ename], 1)
            return body

        block.tensor(run("pe"))
        block.scalar(run("act"))
        block.vector(run("dve"))
        block.gpsimd(run("pool"))
        block.sync(run("sp"))


def chunks(n, w=512):
    return [(c0, min(w, n - c0)) for c0 in range(0, n, w)]


def moff(tb):
    return 128 * (tb * (tb + 1) // 2)


def mtoff(i):
    return 2048 * i - 64 * i * (i - 1)


def build_nc(stop_after=None, dbg=None):
    nc = bass.Bass("TRN2", target_bir_lowering=False)
    dram = lambda n, s, dt=F32, kind="ExternalInput": nc.dram_tensor(n, s, dt, kind=kind).ap()
    x_d = dram("x", [L, D])
    c2_d = dram("c2", [128, 16])
    wada_d = dram("w_ada", [D, 3 * D])
    bsh_d = dram("bsh", [128, 16])
    bsc_d = dram("bsc", [128, 16])
    bgate_d = dram("bgate", [1, D])
    g2_d = dram("g2", [128, 16])
    win_d = dram("w_in", [D, DIN])
    gains_d = dram("gains", [128, 5])
    wout_d = dram("w_out", [D, D])
    identf_d = dram("identf", [128, 128])
    pow2_d = dram("pow2", [128, 32])
    biasc_d = dram("biasc", [128, 152])
    cbf_d = dram("cbf", [128, 1664], BF16)
    out_d = dram("out", [L, D], kind="ExternalOutput")
    dbg_d = None
    if dbg is not None:
        dbg_d = dram("dbg", list(dbg[1]), dbg[2], kind="ExternalOutput")

    S = Sched()
    from contextlib import ExitStack
    es = ExitStack()
    sb = lambda n, s, dt: es.enter_context(nc.sbuf_tensor(n, s, dt))
    with es:
        hT_t = sb("hT", [128, 16 * 2048], BF16)
        R1 = sb("R1", [128, 33792], BF16)
        RP = sb("RP", [128, 8448], BF16)
        stg_t = [sb("stg%d" % i, [128, 16 * 128], F32) for i in range(2)]
        wq_t = sb("wq", [128, 16 * 128], BF16)
        wk_t = sb("wk", [128, 16 * 128], BF16)
        wvz_t = sb("wvz", [128, 16 * 256], BF16)
        RT = sb("RT", [128, 2560], BF16)
        sqt_t = sb("sqt", [128, 512], BF16)
        sqt2_t = sb("sqt2", [128, 512], BF16)
        sd_t = sb("sd", [128, 512], F32)
        bmT_t = sb("bmT", [128, 2048], BF16)
        gatebc = sb("gatebc", [128, D], F32)
        identf = sb("identf_s", [128, 128], F32)
        cbf = sb("cbf_s", [128, 1664], BF16)
        pow2 = sb("pow2_s", [128, 32], F32)
        small = sb("small", [128, 640], F32)
        onesf = sb("onesf", [128, 128], F32)

        ps_t = [es.enter_context(nc.psum_tensor("ps%d" % i, [128, 512], F32)) for i in range(8)]
        PS = [p[:, :] for p in ps_t]

        hT = hT_t[:, :].rearrange("p (a t) -> p a t", a=16)
        qiT = R1[:, 0:16384].rearrange("p (a t) -> p a t", a=8)
        maskv = R1[:, 16384:33792]
        yT = R1[:, 0:32768].rearrange("p (a t) -> p a t", a=16)
        stgA = [R1[:, 0:8192].bitcast(F32).rearrange("p (a n) -> p a n", a=16),
                R1[:, 8192:16384].bitcast(F32).rearrange("p (a n) -> p a n", a=16)]
        modrow = R1[:, 16384:16384 + 12288].bitcast(F32)
        xt = [RP[:, 0:4096].bitcast(F32), RP[:, 4096:8192].bitcast(F32)]
        acc = xt
        qT = RP[:, 0:2048]
        kT = RP[:, 2048:4096]
        szT = RP[:, 4096:6144]
        Vv = RP[:, 6144:8192].rearrange("p (a d) -> p a d", a=16)
        stg = [t[:, :].rearrange("p (a n) -> p a n", a=16) for t in stg_t]
        wq = wq_t[:, :].rearrange("p (a n) -> p a n", a=16)
        wk = wk_t[:, :].rearrange("p (a n) -> p a n", a=16)
        wvz = wvz_t[:, :].rearrange("p (a n) -> p a n", a=16)
        wv = wvz_t[:, 0:2048].rearrange("p (a n) -> p a n", a=16)
        wz = wvz_t[:, 2048:4096].rearrange("p (a n) -> p a n", a=16)
        junk = wvz_t[:, 0:2048]
        mtok = wvz_t[:, 2048:4096]
        junk8 = wvz_t[:, 0:2048].bitcast(mybir.dt.uint8)
        kiT = RT[:, 0:2048]
        wtok = RT[:, 2048:2560].bitcast(F32).rearrange("p (a h) -> p a h", a=16)
        Pt = [RT[:, i * 512:(i + 1) * 512] for i in range(4)]
        ytok = [RT[:, 2048:2176], RT[:, 2176:2304]]
        sqt = sqt_t[:, :]
        sqt2 = sqt2_t[:, :]
        sd = sd_t[:, :]
        bmT = bmT_t[:, :]
        ident_bf = cbf[:, 0:128]
        ones_bf = cbf[:, 128:256]
        blockones = cbf[:, 256:384]
        ctri = cbf[:, 384:512]
        atab = cbf[:, 512:640]
        btab = cbf[:, 640:1152]
        zeros_bf = cbf[:, 1152:1664]
        sc = small[:, 0:16]
        Avec = small[:, 16:32]
        Shv = small[:, 32:48]
        bsh = small[:, 48:64]
        bsc = small[:, 64:80]
        g2 = small[:, 80:96]
        gains = small[:, 96:101]
        gq = small[:, 104:120]
        ss = small[:, 120:121]
        std = small[:, 121:122]
        rstd = small[:, 122:123]
        rmax = small[:, 123:124]
        rmin = small[:, 124:125]
        w0 = small[:, 125:126]
        mid = small[:, 126:127]
        cnt = small[:, 127:128]
        uu = small[:, 128:129]
        thr = small[:, 129:130]
        steps = small[:, 136:168]
        bm = small[:, 168:296].rearrange("p (a n) -> p a n", a=16)
        gt = small[:, 296:304]
        m8 = small[:, 304:312]
        ksum = small[:, 312:320]
        rden = small[:, 320:336]
        c2 = small[:, 336:352]
        epsc = small[:, 368:369]
        biasc = small[:, 400:552]
        tmp16 = small[:, 352:368]
        ksb = cbf_ks = sb("ksb", [128, 8], BF16)[:, :]
        bgrow = R1[0:1, 28672:28672 + 4096].bitcast(F32)

        S.dma(lambda e: e.dma_start(out=identf[:, :], in_=identf_d), writes=["identf"])
        S.dma(lambda e: e.dma_start(out=cbf[:, :], in_=cbf_d), writes=["cbf"])
        S.dma(lambda e: e.dma_start(out=pow2[:, :], in_=pow2_d), writes=["pow2"])
        S.dma(lambda e: e.dma_start(out=biasc, in_=biasc_d), writes=["biasc"])
        S.dma(lambda e: e.dma_start(out=c2, in_=c2_d), writes=["c2"])
        S.dma(lambda e: e.dma_start(out=bsh, in_=bsh_d), writes=["bsh"])
        S.dma(lambda e: e.dma_start(out=bsc, in_=bsc_d), writes=["bsc"])
        S.dma(lambda e: e.dma_start(out=g2, in_=g2_d), writes=["g2"])
        S.dma(lambda e: e.dma_start(out=gains, in_=gains_d), writes=["gains"])
        S.dma(lambda e: e.dma_start(out=bgrow, in_=bgate_d), writes=["bgrow"])
        S.op("pool", lambda e: e.memset(onesf[:, :], 1.0), writes=["onesf"])
        S.op("pool", lambda e: e.memset(epsc, EPS), writes=["epsc"])

        S.op("act", lambda e: e.activation(out=sc, in_=c2, func=AF.Silu), reads=["c2"], writes=["sc"])
        if stop_after == "A1":
            return finish(nc, S, es, dbg, dbg_d, locals())
        wada_v = wada_d.rearrange("(kh kl) n -> kl kh n", kl=128)
        NA = 24
        for ci in range(NA):
            sl = ci % 2
            c0 = ci * 256
            S.dma(lambda e, sl=sl, c0=c0: e.dma_start(out=stgA[sl], in_=wada_v[:, :, c0:c0 + 256]),
                  writes=[("stgA", sl)])
            pb = ci % 2
            for kh in range(16):
                S.op("pe", lambda e, sl=sl, kh=kh, pb=pb: e.matmul(
                    out=PS[pb][0:1, 0:256], lhsT=sc[:, kh:kh + 1], rhs=stgA[sl][:, kh, :],
                    start=(kh == 0), stop=(kh == 15)),
                    reads=[("stgA", sl), "sc"], writes=[("ps", pb)])
            S.op("dve", lambda e, c0=c0, pb=pb: e.tensor_copy(out=modrow[0:1, c0:c0 + 256], in_=PS[pb][0:1, 0:256]),
                 reads=[("ps", pb)], writes=["modrow"])
        if stop_after == "A2":
            return finish(nc, S, es, dbg, dbg_d, locals())
        for j in range(32):
            S.op("pe", lambda e, j=j: e.matmul(out=PS[2][:, j:j + 1], lhsT=modrow[0:1, j * 128:(j + 1) * 128],
                                               rhs=identf[0:1, 0:1], start=True, stop=True),
                 reads=["modrow", "identf"], writes=[("ps", 2)])
        S.op("dve", lambda e: e.tensor_tensor(out=Shv, in0=PS[2][:, 0:16], in1=bsh, op=ALU.add),
             reads=[("ps", 2), "bsh"], writes=["Shv"])
        S.op("dve", lambda e: e.tensor_tensor(out=tmp16, in0=PS[2][:, 16:32], in1=bsc, op=ALU.add),
             reads=[("ps", 2), "bsc"], writes=["tmp16"])
        S.op("dve", lambda e: e.scalar_tensor_tensor(out=Avec, in0=tmp16, scalar=1.0, in1=g2, op0=ALU.add, op1=ALU.mult),
             reads=["tmp16", "g2"], writes=["Avec"])
        if stop_after == "A3":
            return finish(nc, S, es, dbg, dbg_d, locals())
        S.op("dve", lambda e: e.tensor_tensor(out=modrow[0:1, 4096:6144], in0=modrow[0:1, 4096:6144], in1=bgrow,
                                              op=ALU.add), reads=["modrow", "bgrow"], writes=["modrow"])
        for q in range(4):
            pb = 3 + (q % 2)
            S.op("pe", lambda e, q=q, pb=pb: e.matmul(out=PS[pb][:, :], lhsT=onesf[0:1, 0:128],
                                                      rhs=modrow[0:1, 4096 + q * 512:4096 + (q + 1) * 512],
                                                      start=True, stop=True),
                 reads=["modrow", "onesf"], writes=[("ps", pb)])
            S.op("act", lambda e, q=q, pb=pb: e.activation(out=gatebc[:, q * 512:(q + 1) * 512], in_=PS[pb][:, :],
                                                           func=AF.Identity),
                 reads=[("ps", pb)], writes=["gatebc"])
        for u in range(16):
            h = u % 8
            col = 0 if u < 8 else 3
            fac = (128.0 ** -0.5) * (2.0 ** (h + 1))
            S.op("dve", lambda e, u=u, col=col, fac=fac: e.tensor_scalar(
                out=gq[:, u:u + 1], in0=gains[:, col:col + 1], scalar1=fac, scalar2=None, op0=ALU.mult),
                reads=["gains"], writes=["gq"])
        S.barrier()
        if stop_after == "A":
            return finish(nc, S, es, dbg, dbg_d, locals())

        for tb in range(NB):
            sl = tb % 2
            S.dma(lambda e, sl=sl, tb=tb: e.dma_start(out=xt[sl], in_=x_d[tb * 128:(tb + 1) * 128, :]),
                  writes=[("xt", sl)])
            S.op("act", lambda e, sl=sl: e.activation(out=junk, in_=xt[sl], func=AF.Square, accum_out=ss),
                 reads=[("xt", sl)], writes=["junk", "ss"])
            S.op("act", lambda e: e.activation(out=std, in_=ss, func=AF.Sqrt, bias=EPS, scale=1.0 / D),
                 reads=["ss"], writes=["std"])
            S.op("dve", lambda e: e.reciprocal(out=rstd, in_=std), reads=["std"], writes=["rstd"])
            S.op("dve", lambda e, sl=sl: e.tensor_scalar(out=xt[sl], in0=xt[sl], scalar1=rstd, scalar2=None,
                                                         op0=ALU.mult),
                 reads=["rstd", ("xt", sl)], writes=[("xt", sl)])
            if stop_after == "B1":
                return finish(nc, S, es, dbg, dbg_d, locals())
            for g in range(4):
                pb = g
                if stop_after == "B3" and g == 1:
                    return finish(nc, S, es, dbg, dbg_d, locals())
                for qd in range(4):
                    dh = g * 4 + qd
                    S.op("pe", lambda e, sl=sl, dh=dh, pb=pb, qd=qd: e.transpose(
                        out=PS[pb][:, qd * 128:(qd + 1) * 128], in_=xt[sl][:, dh * 128:(dh + 1) * 128],
                        identity=identf[:, :]),
                        reads=[("xt", sl), "identf"], writes=[("ps", pb)])
                if stop_after == "B2":
                    return finish(nc, S, es, dbg, dbg_d, locals())
                for qd in range(4):
                    dh = g * 4 + qd
                    if qd % 2 == 0:
                        S.op("act", lambda e, tb=tb, dh=dh, pb=pb, qd=qd: e.activation(
                            out=hT[:, dh, tb * 128:(tb + 1) * 128], in_=PS[pb][:, qd * 128:(qd + 1) * 128],
                            func=AF.Identity, bias=Shv[:, dh:dh + 1], scale=Avec[:, dh:dh + 1]),
                            reads=[("ps", pb), "Shv", "Avec"], writes=[("hT", tb)])
                    else:
                        S.op("dve", lambda e, tb=tb, dh=dh, pb=pb, qd=qd: e.tensor_scalar(
                            out=hT[:, dh, tb * 128:(tb + 1) * 128], in0=PS[pb][:, qd * 128:(qd + 1) * 128],
                            scalar1=Avec[:, dh:dh + 1], scalar2=Shv[:, dh:dh + 1], op0=ALU.mult, op1=ALU.add),
                            reads=[("ps", pb), "Shv", "Avec"], writes=[("hT", tb)])
        S.barrier()
        if stop_after == "B":
            return finish(nc, S, es, dbg, dbg_d, locals())

        win_v = win_d.rearrange("(dh dl) n -> dl dh n", dl=128)
        state = {"stg": 0, "ps": 0, "sbank": 0, "pslot": 0}

        def load_slab(c0, ncol, casts):
            sl = state["stg"] % 2
            state["stg"] += 1
            S.dma(lambda e, sl=sl: e.dma_start(out=stg[sl][:, :, 0:ncol], in_=win_v[:, :, c0:c0 + ncol]),
                  writes=[("stg", sl)])
            for (dst, s0, s1, key) in casts:
                S.op("pool", lambda e, sl=sl, dst=dst, s0=s0, s1=s1: e.tensor_copy(out=dst, in_=stg[sl][:, :, s0:s1]),
                     reads=[("stg", sl)], writes=[key])

        def next_ps(n=2, base=0):
            b = base + state["ps"] % n
            state["ps"] += 1
            return b

        def proj_fm(wslab, wkey, tc, pb):
            for dh in range(16):
                S.op("pe", lambda e, dh=dh: e.matmul(out=PS[pb][:, :], lhsT=wslab[:, dh, :],
                                                     rhs=hT[:, dh, tc * 512:(tc + 1) * 512],
                                                     start=(dh == 0), stop=(dh == 15)),
                     reads=[wkey] + [("hT", tc * 4 + i) for i in range(4)], writes=[("ps", pb)])

        sqs = [sqt, sqt2]

        def norm_a(pb, qi_):
            S.op("act", lambda e: e.activation(out=sqs[qi_], in_=PS[pb][:, :], func=AF.Square),
                 reads=[("ps", pb)], writes=[("sqt", qi_)])

        def norm_b(pb, pb2, qi_, onesmat, ndim, gcol, dst, dkey):
            S.op("pe", lambda e: e.matmul(out=PS[pb2][:, :], lhsT=onesmat, rhs=sqs[qi_], start=True, stop=True),
                 reads=[("sqt", qi_), "cbf"], writes=[("ps", pb2)])
            S.op("act", lambda e: e.activation(out=sd, in_=PS[pb2][:, :], func=AF.Ln, bias=epsc, scale=1.0 / ndim),
                 reads=[("ps", pb2), "epsc"], writes=["sd"])
            S.op("act", lambda e: e.activation(out=sd, in_=sd, func=AF.Exp, scale=-0.5),
                 reads=["sd"], writes=["sd"])
            S.op("dve", lambda e: e.scalar_tensor_tensor(out=dst, in0=PS[pb][:, :], scalar=gcol, in1=sd,
                                                         op0=ALU.mult, op1=ALU.mult),
                 reads=[("ps", pb), "sd", "gains", "gq"], writes=[dkey])

        def norm_evac(pb, pb2, onesmat, ndim, gcol, dst, dkey):
            norm_a(pb, 0)
            norm_b(pb, pb2, 0, onesmat, ndim, gcol, dst, dkey)

        def proj_norm_pipe(jobs):
            prev = None
            for n_, (w_, wk_, tc, gcol, dst, dkey) in enumerate(jobs):
                pb = n_ % 3
                proj_fm(w_, wk_, tc, pb)
                norm_a(pb, n_ % 2)
                if prev is not None:
                    norm_b(*prev)
                prev = (pb, 3, n_ % 2, ones_bf, 128.0, gcol, dst, dkey)
            norm_b(*prev)

        for p in range(8):
            load_slab(4096 + 128 * p, 128, [(wq, 0, 128, "wq")])
            for tc in range(4):
                pb = next_ps(2, 0)
                proj_fm(wq, "wq", tc, pb)
                S.op("act", lambda e, p=p, tc=tc, pb=pb: e.activation(
                    out=qiT[:, p, tc * 512:(tc + 1) * 512], in_=PS[pb][:, :], func=AF.Identity),
                    reads=[("ps", pb)], writes=[("qiT", p)])
        load_slab(5120, 80, [(wk[:, :, 0:64], 0, 64, "wk"), (wk[:, :, 64:128], 0, 64, "wk"),
                             (wvz[:, :, 0:16], 64, 80, "wvz")])
        for tc in range(4):
            pb = next_ps(2, 0)
            proj_fm(wk, "wk", tc, pb)
            norm_evac(pb, 2 + tc % 2, blockones, 64.0, gains[:, 2:3], kiT[:, tc * 512:(tc + 1) * 512], ("kiT", tc))
        for tb in range(NB):
            pb = 4 + tb % 2
            for dh in range(16):
                S.op("pe", lambda e, tb=tb, dh=dh, pb=pb: e.matmul(
                    out=PS[pb][:, 0:16], lhsT=hT[:, dh, tb * 128:(tb + 1) * 128], rhs=wvz[:, dh, 0:16],
                    start=(dh == 0), stop=(dh == 15)),
                    reads=["wvz", ("hT", tb)], writes=[("ps", pb)])
            S.op("dve", lambda e, tb=tb, pb=pb: e.tensor_copy(out=wtok[:, tb, :], in_=PS[pb][:, 0:16]),
                 reads=[("ps", pb)], writes=[("wtok", tb)])
        S.barrier()
        if stop_after == "C":
            return finish(nc, S, es, dbg, dbg_d, locals())

        psr = 0
        kiT_hi = wq_t[:, 0:2048]
        kkeys = [("kiT", i) for i in range(4)]
        S.op("pool", lambda e: e.tensor_copy(out=kiT_hi[64:128, :], in_=kiT[64:128, :]), reads=kkeys, writes=["kiThi"])
        S.op("pool", lambda e: e.memset(kiT_hi[0:64, :], 0.0), writes=["kiThi"])
        S.op("pool", lambda e: e.memset(kiT[64:128, :], 0.0), reads=["kiThi"], writes=kkeys)
        dgs = [bmT_t[:, :].rearrange("p (h n) -> p h n", h=16), wk_t[:, :].rearrange("p (h n) -> p h n", h=16)]
        sd_bf = sd_t[:, :].bitcast(BF16)
        rts = [sqt, sqt2, sd_bf[:, 0:512], sd_bf[:, 512:1024]]
        dstate = {"lb": 0, "rs": 0, "sc": 0}
        accs = [xt[0], xt[1], stg_t[0][:, :], stg_t[1][:, :]]
        scr = [dict(rmax=rmax, rmin=rmin, w0=w0, mid=mid, cnt=cnt, uu=uu, thr=thr, steps=steps[:, 0:NIT + 1], tag="0"),
               dict(rmax=small[:, 369:370], rmin=small[:, 370:371], w0=small[:, 371:372], mid=small[:, 372:373],
                    cnt=small[:, 373:374], uu=small[:, 374:375], thr=small[:, 375:376], steps=small[:, 376:376 + NIT + 1],
                    tag="1")]

        def build_dg(tb):
            dg = dgs[tb % 2]
            S.op("dve", lambda e: e.tensor_tensor(
                out=dg, in0=ident_bf.unsqueeze(1).to_broadcast([128, 16, 128]),
                in1=wtok[:, tb, :].unsqueeze(2).to_broadcast([128, 16, 128]), op=ALU.mult),
                reads=[("wtok", tb), "cbf"], writes=[("dg", tb % 2)])

        def do_scores(tb, a, akey):
            ncol = 128 * (tb + 1)
            dg = dgs[tb % 2]
            dgk = ("dg", tb % 2)
            for (c0, cw) in chunks(ncol):
                scb = 4 + dstate["sc"] % 2
                dstate["sc"] += 1

                def emit_L(h, c0=c0, cw=cw, tb=tb):
                    pb = dstate["lb"] % 4
                    dstate["lb"] += 1
                    ksrc = kiT if h % 2 == 0 else kiT_hi
                    kk = kkeys if h % 2 == 0 else ["kiThi"]
                    S.op("pe", lambda e: e.matmul(
                        out=PS[pb][:, 0:cw], lhsT=qiT[:, h // 2, tb * 128:(tb + 1) * 128], rhs=ksrc[:, c0:c0 + cw],
                        start=True, stop=True),
                        reads=[("qiT", h // 2)] + kk, writes=[("ps", pb)])
                    return pb

                def emit_relu(h, pb, cw=cw):
                    rs_ = dstate["rs"] % 4
                    dstate["rs"] += 1
                    S.op("act", lambda e: e.activation(out=rts[rs_][:, 0:cw], in_=PS[pb][:, 0:cw], func=AF.Relu),
                         reads=[("ps", pb)], writes=[("rt", rs_)])
                    return rs_

                def emit_acc(h, rs_, cw=cw, scb=scb, dg=dg, dgk=dgk):
                    S.op("pe", lambda e: e.matmul(
                        out=PS[scb][:, 0:cw], lhsT=dg[:, h, :], rhs=rts[rs_][:, 0:cw], start=(h == 0), stop=(h == 15)),
                        reads=[("rt", rs_), dgk], writes=[("ps", scb)])

                LA = 2
                pbs = {}
                for h in range(LA):
                    pbs[h] = emit_L(h)
                for h in range(16):
                    if h + LA < 16:
                        pbs[h + LA] = emit_L(h + LA)
                    rs_ = emit_relu(h, pbs[h])
                    emit_acc(h, rs_)
                S.op("act", lambda e, a=a, c0=c0, cw=cw, scb=scb: e.activation(out=a[:, c0:c0 + cw], in_=PS[scb][:, 0:cw],
                                                                               func=AF.Identity),
                     reads=[("ps", scb)], writes=[akey])

        def prep(tb, a, akey, sc):
            ncol = 128 * (tb + 1)
            t = sc["tag"]
            if tb >= 2:
                S.op("dve", lambda e: e.tensor_reduce(out=sc["rmax"], in_=a[:, 0:ncol], axis=AX.X, op=ALU.max),
                     reads=[akey], writes=["rmax" + t])
                S.op("dve", lambda e: e.tensor_reduce(out=sc["rmin"], in_=a[:, 0:ncol], axis=AX.X, op=ALU.min),
                     reads=[akey], writes=["rmin" + t])
            S.op("pool", lambda e: e.affine_select(
                out=a[:, tb * 128:(tb + 1) * 128], in_=a[:, tb * 128:(tb + 1) * 128], pattern=[[-1, 128]],
                compare_op=ALU.is_ge, fill=NEG, base=0, channel_multiplier=1),
                reads=[akey], writes=[akey])
            if tb >= 2:
                S.op("dve", lambda e: e.tensor_tensor(out=sc["w0"], in0=sc["rmax"], in1=sc["rmin"], op=ALU.subtract),
                     reads=["rmax" + t, "rmin" + t], writes=["w0" + t])
                S.op("dve", lambda e: e.tensor_scalar(out=sc["steps"], in0=pow2[:, 0:NIT + 1], scalar1=sc["w0"], scalar2=None,
                                                      op0=ALU.mult),
                     reads=["w0" + t, "pow2"], writes=["steps" + t])
                S.op("dve", lambda e: e.tensor_tensor(out=sc["mid"], in0=sc["rmin"], in1=sc["steps"][:, 0:1], op=ALU.add),
                     reads=["rmin" + t, "steps" + t], writes=["mid" + t])
            else:
                S.op("dve", lambda e: e.memset(sc["thr"], -1.0e29), writes=["thr" + t])

        def it_pass(tb, a, akey, sc, k):
            ncol = 128 * (tb + 1)
            t = sc["tag"]
            jk = junk8[:, 0:ncol] if t == "0" else junk8[:, 2048:2048 + ncol]
            S.op("dve", lambda e: e.tensor_scalar(
                out=jk, in0=a[:, 0:ncol], scalar1=sc["mid"], scalar2=None, op0=ALU.is_ge, op1=ALU.add,
                accum_out=sc["cnt"]),
                reads=[akey, "mid" + t], writes=["cnt" + t, "junk" + t])

        def it_u(tb, a, akey, sc, k):
            t = sc["tag"]
            S.op("dve", lambda e: e.tensor_scalar(out=sc["uu"], in0=sc["cnt"], scalar1=TOPK - 0.5, scalar2=0.5,
                                                  op0=ALU.is_gt, op1=ALU.subtract),
                 reads=["cnt" + t], writes=["uu" + t])

        def it_mid(tb, a, akey, sc, k):
            t = sc["tag"]
            S.op("dve", lambda e: e.scalar_tensor_tensor(out=sc["mid"], in0=sc["uu"], scalar=sc["steps"][:, k:k + 1],
                                                         in1=sc["mid"], op0=ALU.mult, op1=ALU.add),
                 reads=["uu" + t, "steps" + t, "mid" + t], writes=["mid" + t])

        def fin(tb, a, akey, sc):
            nonlocal psr
            ncol = 128 * (tb + 1)
            t = sc["tag"]
            if tb >= 2:
                S.op("dve", lambda e: e.tensor_tensor(out=sc["thr"], in0=sc["mid"], in1=sc["steps"][:, NIT:NIT + 1],
                                                      op=ALU.subtract),
                     reads=["mid" + t, "steps" + t], writes=["thr" + t])
            S.op("dve", lambda e: e.tensor_scalar(
                out=mtok[:, 0:ncol], in0=a[:, 0:ncol], scalar1=sc["thr"], scalar2=MASKV, op0=ALU.is_lt, op1=ALU.mult),
                reads=[akey, "thr" + t], writes=["mtok"])
            for g0 in range(0, tb + 1, 4):
                pb = 6 + psr % 2
                psr += 1
                blk = list(range(g0, min(g0 + 4, tb + 1)))
                for qi_, i in enumerate(blk):
                    S.op("pe", lambda e, qi_=qi_, i=i, pb=pb: e.transpose(
                        out=PS[pb][:, qi_ * 64:(qi_ + 1) * 64].bitcast(BF16), in_=mtok[:, i * 128:(i + 1) * 128],
                        identity=ident_bf),
                        reads=["mtok", "cbf"], writes=[("ps", pb)])
                for qi_, i in enumerate(blk):
                    mo = mtoff(i) + (tb - i) * 128
                    S.op("act", lambda e, qi_=qi_, mo=mo, pb=pb: e.activation(
                        out=maskv[:, mo:mo + 128], in_=PS[pb][:, qi_ * 64:(qi_ + 1) * 64].bitcast(BF16), func=AF.Identity),
                        reads=[("ps", pb)], writes=[("maskT", i)])

        for pi in range(8):
            items = []
            for q_ in range(2):
                tb = 2 * pi + q_
                ai = (2 * pi + q_) % 4
                items.append((tb, accs[ai], ("acc", ai), scr[q_]))
            if pi == 0:
                build_dg(0)
                build_dg(1)
            for it in items:
                do_scores(it[0], it[1], it[2])
            if pi < 7:
                build_dg(2 * pi + 2)
                build_dg(2 * pi + 3)
            for it in items:
                prep(*it)
            if pi >= 1:
                for k in range(NIT):
                    for fnk in (it_pass, it_u, it_mid):
                        for it in items:
                            fnk(*it, k)
            for it in items:
                fin(*it)
        S.barrier()
        if stop_after == "D":
            return finish(nc, S, es, dbg, dbg_d, locals())

        S.op("pool", lambda e: e.memset(bmT, 0.0), writes=["bmT"])
        QB = {0: (0, 1024, 2048, 3072), 1: (5200, 6224, 7248, 8272)}
        mbt = small[:, 560:624].bitcast(BF16)

        def load_unit(u):
            grp, h = u // 8, u % 8
            qb, kb, vb, zb = QB[grp]
            load_slab(qb + 128 * h, 128, [(wq, 0, 128, "wq")])
            load_slab(kb + 128 * h, 128, [(wk, 0, 128, "wk")])
            load_slab(vb + 128 * h, 128, [(wv, 0, 128, "wv")])
            load_slab(zb + 128 * h, 128, [(wz, 0, 128, "wz")])

        load_unit(UNITS[0])
        for ui, u in enumerate(UNITS):
            grp, h = u // 8, u % 8
            slope = 2.0 ** (-(h + 1))
            kcol = 1 if grp == 0 else 4
            proj_norm_pipe([(wq, "wq", tc, gq[:, u:u + 1], qT[:, tc * 512:(tc + 1) * 512], ("qT", tc)) for tc in range(4)] +
                           [(wk, "wk", tc, gains[:, kcol:kcol + 1], kT[:, tc * 512:(tc + 1) * 512], ("kT", tc))
                            for tc in range(4)])
            if stop_after == "E1" and ui == STOP_UI:
                return finish(nc, S, es, dbg, dbg_d, locals())
            if grp == 1:
                S.op("dve", lambda e: e.tensor_reduce(out=ksum, in_=kT.rearrange("p (n s) -> p n s", n=8), axis=AX.X,
                                                      op=ALU.add),
                     reads=[("kT", i) for i in range(4)], writes=["ksum"])
                S.op("dve", lambda e: e.tensor_copy(out=ksb, in_=ksum), reads=["ksum"], writes=["ksb"])
                for tb in range(NB):
                    S.op("pe", lambda e, tb=tb: e.matmul(out=PS[7][:, tb * 8:(tb + 1) * 8], lhsT=qT[:, tb * 128:(tb + 1) * 128],
                                                         rhs=ksb, start=True, stop=True),
                         reads=[("qT", tb // 4), "ksb"], writes=[("ps", 7)])
                for tb in range(NB):
                    n = tb // 2
                    S.op("dve", lambda e, tb=tb: e.tensor_copy(out=gt, in_=PS[7][:, tb * 8:(tb + 1) * 8]),
                         reads=[("ps", 7)], writes=["gt"])
                    if n < 8:
                        S.op("dve", lambda e, n=n: e.memset(gt[:, n:8], NEG), reads=["gt"], writes=["gt"])
                    S.op("dve", lambda e: e.max(out=m8, in_=gt), reads=["gt"], writes=["m8"])
                    S.op("dve", lambda e, tb=tb: e.tensor_scalar(out=mbt[:, tb * 8:(tb + 1) * 8], in0=gt, scalar1=m8[:, 2:3],
                                                                 scalar2=MASKV, op0=ALU.is_lt, op1=ALU.mult),
                         reads=["gt", "m8"], writes=["mbt"])
                for g in range(2):
                    pb = next_ps(2, 0)
                    for q_ in range(8):
                        tb = 8 * g + q_
                        S.op("pe", lambda e, tb=tb, q_=q_, pb=pb: e.transpose(
                            out=PS[pb][0:8, q_ * 64:(q_ + 1) * 64].bitcast(BF16), in_=mbt[:, tb * 8:(tb + 1) * 8],
                            identity=ident_bf),
                            reads=["mbt", "cbf"], writes=[("ps", pb)])
                    S.op("act", lambda e, g=g, pb=pb: e.activation(
                        out=bmT[0:8, g * 1024:(g + 1) * 1024], in_=PS[pb][0:8, :].bitcast(BF16), func=AF.Identity),
                        reads=[("ps", pb)], writes=["bmT"])
            for g in range(4):
                pb = next_ps(2, 0)
                for q_ in range(4):
                    tb = 4 * g + q_
                    for dh in range(16):
                        S.op("pe", lambda e, tb=tb, dh=dh, pb=pb, q_=q_: e.matmul(
                            out=PS[pb][:, q_ * 128:(q_ + 1) * 128], lhsT=hT[:, dh, tb * 128:(tb + 1) * 128],
                            rhs=wv[:, dh, :], start=(dh == 0), stop=(dh == 15)),
                            reads=["wv", ("hT", tb)], writes=[("ps", pb)])
                S.op("dve", lambda e, g=g, pb=pb: e.tensor_copy(
                    out=RP[:, 6144 + g * 512:6144 + (g + 1) * 512], in_=PS[pb][:, :]),
                    reads=[("ps", pb)], writes=[("V", g)])
            for tc in range(4):
                pb = next_ps(2, 0)
                proj_fm(wz, "wz", tc, pb)
                S.op("act", lambda e, tc=tc, pb=pb: e.activation(out=szT[:, tc * 512:(tc + 1) * 512], in_=PS[pb][:, :],
                                                                 func=AF.Silu),
                     reads=[("ps", pb)], writes=[("szT", tc)])
            if stop_after == "E2" and ui == STOP_UI:
                return finish(nc, S, es, dbg, dbg_d, locals())
            if ui + 1 < len(UNITS):
                load_unit(UNITS[ui + 1])
            pslot = 0
            sbank = 0
            for j in range(4):
                tb0 = 4 * j
                o1 = 4 + 2 * (j % 2)
                o2 = 5 + 2 * (j % 2)
                ni = 4 * j + 4
                def emit_S(i):
                    nonlocal_state = state
                    tlo = max(tb0, i)
                    c0 = (tlo - tb0) * 128
                    sb_ = state["sbank"] % 4
                    state["sbank"] += 1
                    nprime = i // 2
                    mm = []
                    mm.append((kT[:, i * 128:(i + 1) * 128], qT[:, tlo * 128:(tb0 + 4) * 128], c0, 512,
                               [("kT", i // 4), ("qT", j)]))
                    mm.append((atab, btab[:, c0:512], c0, 512, ["cbf"]))
                    if grp == 0:
                        mo = mtoff(i) + (tlo - i) * 128
                        mm.append((ident_bf, maskv[:, mo:mo + 512 - c0], c0, 512, [("maskT", i), "cbf"]))
                    else:
                        gl = max(tlo, 2 * nprime + 2)
                        if gl < tb0 + 4:
                            gc = (gl - tb0) * 128
                            mm.append((ident_bf[:, nprime:nprime + 1].to_broadcast([128, 128]),
                                       bmT[:, gl * 128:(tb0 + 4) * 128], gc, 512, ["bmT", "cbf"]))
                        if i >= tb0:
                            dc_ = (i - tb0) * 128
                            mm.append((ident_bf, ctri, dc_, dc_ + 128, ["cbf"]))
                    for mi, (lh, rh, a_, b_, rk) in enumerate(mm):
                        S.op("pe", lambda e, lh=lh, rh=rh, a_=a_, b_=b_, sb_=sb_, mi=mi, last=(mi == len(mm) - 1): e.matmul(
                            out=PS[sb_][:, a_:b_], lhsT=lh, rhs=rh, start=(mi == 0), stop=last),
                            reads=rk, writes=[("ps", sb_)])
                    return sb_, c0

                def emit_exp(i, sb_, c0):
                    ps_ = state["pslot"] % 4
                    state["pslot"] += 1
                    bcol = h * 19 + (i - tb0 + 15)
                    S.op("act", lambda e, bcol=bcol, slope=slope: e.activation(
                        out=Pt[ps_][:, c0:512], in_=PS[sb_][:, c0:512], func=AF.Exp,
                        bias=biasc[:, bcol:bcol + 1], scale=slope),
                        reads=[("ps", sb_), "biasc"], writes=[("P", ps_)])
                    return ps_

                def emit_PV(i, ps_, c0):
                    S.op("pe", lambda e, o1=o1, ni=ni: e.matmul(
                        out=PS[o1][:, c0:512], lhsT=Vv[:, i, :], rhs=Pt[ps_][:, c0:512], start=(i == 0), stop=(i == ni - 1)),
                        reads=[("P", ps_), ("V", i // 4)], writes=[("ps", o1)])
                    S.op("pe", lambda e, o2=o2, ni=ni: e.matmul(
                        out=PS[o2][:, c0:512], lhsT=ones_bf, rhs=Pt[ps_][:, c0:512], start=(i == 0), stop=(i == ni - 1)),
                        reads=[("P", ps_), "cbf"], writes=[("ps", o2)])

                LA = 2
                info = {}
                for i in range(min(LA, ni)):
                    info[i] = emit_S(i)
                for i in range(ni):
                    if i + LA < ni:
                        info[i + LA] = emit_S(i + LA)
                    ps_ = emit_exp(i, *info[i])
                    emit_PV(i, ps_, info[i][1])
                if stop_after == "E3" and ui == STOP_UI:
                    return finish(nc, S, es, dbg, dbg_d, locals())
                S.op("act", lambda e, o2=o2: e.activation(out=sd, in_=PS[o2][:, :], func=AF.Ln), reads=[("ps", o2)],
                     writes=["sd"])
                S.op("act", lambda e: e.activation(out=sd, in_=sd, func=AF.Exp, scale=-1.0), reads=["sd"], writes=["sd"])
                S.op("dve", lambda e, j=j: e.tensor_tensor(out=sd, in0=sd, in1=szT[:, j * 512:(j + 1) * 512], op=ALU.mult),
                     reads=["sd", ("szT", j)], writes=["sd"])
                S.op("dve", lambda e, u=u, j=j, o1=o1: e.tensor_tensor(
                    out=yT[:, u, j * 512:(j + 1) * 512], in0=PS[o1][:, :], in1=sd, op=ALU.mult),
                    reads=[("ps", o1), "sd"], writes=[("yT", 4 * j), ("yT", 4 * j + 1), ("yT", 4 * j + 2), ("yT", 4 * j + 3)])
            if stop_after == "E4" and ui == STOP_UI:
                return finish(nc, S, es, dbg, dbg_d, locals())
            if u == 7:
                S.barrier()
        S.barrier()
        if stop_after == "E":
            return finish(nc, S, es, dbg, dbg_d, locals())

        wout_v = wout_d.rearrange("(mh ml) n -> ml mh n", ml=128)
        wob = hT
        def wout_chunks(dc):
            for cc in range(4 * dc, 4 * dc + 4):
                sl = state["stg"] % 2
                state["stg"] += 1
                c0 = cc * 128
                S.dma(lambda e, sl=sl, c0=c0: e.dma_start(out=stg[sl], in_=wout_v[:, :, c0:c0 + 128]), writes=[("stg", sl)])
                for mh in range(16):
                    S.op("pool" if mh % 2 == 0 else "dve", lambda e, sl=sl, c0=c0, mh=mh: e.tensor_tensor(
                        out=wob[:, mh, c0:c0 + 128], in0=stg[sl][:, mh, :], in1=gatebc[:, c0:c0 + 128], op=ALU.mult),
                        reads=[("stg", sl), "gatebc"], writes=[("wob", dc)])

        xos = [xt[0][:, i * 512:(i + 1) * 512] for i in range(4)]
        ots = [xt[1][:, i * 512:(i + 1) * 512] for i in range(4)]
        out_toks = []
        wout_chunks(0)
        nt = 0
        for dc in range(4):
            if dc + 1 < 4:
                wout_chunks(dc + 1)
            for tb in range(NB):
                sl4 = nt % 4
                nt += 1
                S.dma(lambda e, tb=tb, dc=dc, sl4=sl4: e.dma_start(
                    out=xos[sl4], in_=x_d[tb * 128:(tb + 1) * 128, dc * 512:(dc + 1) * 512]), writes=[("xo", sl4)])
                pb = next_ps(4, 0)
                for mh in range(16):
                    S.op("pe", lambda e, tb=tb, dc=dc, mh=mh, pb=pb: e.matmul(
                        out=PS[pb][:, :], lhsT=yT[:, mh, tb * 128:(tb + 1) * 128], rhs=wob[:, mh, dc * 512:(dc + 1) * 512],
                        start=(mh == 0), stop=(mh == 15)),
                        reads=[("yT", tb), ("wob", dc)], writes=[("ps", pb)])
                S.op("dve", lambda e, sl4=sl4, pb=pb: e.tensor_tensor(
                    out=ots[sl4], in0=PS[pb][:, :], in1=xos[sl4], op=ALU.add),
                    reads=[("ps", pb), ("xo", sl4)], writes=[("ot", sl4)])
                out_toks.append(S.dma(lambda e, tb=tb, dc=dc, sl4=sl4: e.dma_start(
                    out=out_d[tb * 128:(tb + 1) * 128, dc * 512:(dc + 1) * 512], in_=ots[sl4]),
                    reads=[("ot", sl4)], writes=[("outd", tb, dc)]))
        S.ops["sp"].append((None, out_toks, None))
        return finish(nc, S, es, None, None, locals())


def finish(nc, S, es, dbg, dbg_d, env):
    if dbg is not None:
        src = env[dbg[0]]
        S.barrier()
        t = S.dma(lambda e: e.dma_start(out=dbg_d, in_=dbg[3](env)), reads=[], writes=["dbgout"])
        S.ops["sp"].append((None, [t], None))
    from contextlib import ExitStack
    with ExitStack() as es2:
        sems = {e: es2.enter_context(nc.semaphore("sem_" + e)) for e in ENGS if e != "sp"}
        dsems = [es2.enter_context(nc.semaphore("dsem%d" % i)) for i in range(NDMASEM)]
        block = es2.enter_context(nc.Block())
        S.emit(nc, block, sems, dsems)
    return nc


def _consts():
    identf = np.eye(128, dtype=np.float32)
    pow2 = np.tile((2.0 ** -(np.arange(32) + 1.0)).astype(np.float32)[None, :], (128, 1))
    cb = np.zeros((128, 1664), dtype=np.float32)
    cb[:, 0:128] = np.eye(128)
    cb[:, 128:256] = 1.0
    bo = np.zeros((128, 128), np.float32)
    bo[:64, :64] = 1.0
    bo[64:, 64:] = 1.0
    cb[:, 256:384] = bo
    sidx = np.arange(128)[:, None]
    tidx = np.arange(128)[None, :]
    cb[:, 384:512] = np.where(sidx > tidx, MASKV, 0.0)
    at = np.zeros((128, 128), np.float32)
    at[0, :] = np.arange(128)
    at[1, :] = 1.0
    at[2, :] = 1.0
    cb[:, 512:640] = at
    bt = np.zeros((128, 512), np.float32)
    tr = np.arange(512)
    bt[0, :] = 1.0
    bt[1, :] = -128.0 * (tr // 128)
    bt[2, :] = -(tr % 128)
    cb[:, 640:1152] = bt
    bc = np.zeros((128, 152), np.float32)
    for h in range(8):
        for v in range(-15, 4):
            bc[:, h * 19 + v + 15] = (2.0 ** -(h + 1)) * 128.0 * v
    return identf, pow2, cb.astype(ml_dtypes.bfloat16), bc


def _in_maps(inputs):
    x = np.asarray(inputs["x"], np.float32)
    c = np.asarray(inputs["c"], np.float32)
    w_ada = np.ascontiguousarray(np.asarray(inputs["w_ada"], np.float32)[0])
    b_ada = np.asarray(inputs["b_ada"], np.float32)[0]
    g_norm = np.asarray(inputs["g_norm"], np.float32)[0]
    w_in = np.ascontiguousarray(np.asarray(inputs["w_in"], np.float32)[0])
    w_out = np.ascontiguousarray(np.asarray(inputs["w_out"], np.float32)[0])
    lay = lambda v: np.ascontiguousarray(v.reshape(16, 128).T)
    kni = np.asarray(inputs["k_norm_idx"], np.float32)[0]
    gains = np.stack([np.asarray(inputs["q_norm_a"], np.float32)[0], np.asarray(inputs["k_norm_a"], np.float32)[0],
                      np.concatenate([kni, kni]), np.asarray(inputs["q_norm_b"], np.float32)[0],
                      np.asarray(inputs["k_norm_b"], np.float32)[0]], axis=1).astype(np.float32)
    identf, pow2, cbf, biasc = _consts()
    maps = []
    for b in range(8):
        maps.append({
            "x": np.ascontiguousarray(x[b]), "c2": lay(c[b]), "w_ada": w_ada,
            "bsh": lay(b_ada[0:D]), "bsc": lay(b_ada[D:2 * D]), "bgate": np.ascontiguousarray(b_ada[2 * D:3 * D][None, :]),
            "g2": lay(g_norm), "w_in": w_in, "gains": np.ascontiguousarray(gains), "w_out": w_out,
            "identf": identf, "pow2": pow2, "cbf": cbf, "biasc": biasc,
        })
    return maps


_NC_CACHE = {}


def kernel(**inputs):
    if "nc" not in _NC_CACHE:
        _NC_CACHE["nc"] = build_nc()
    nc = _NC_CACHE["nc"]
    maps = _in_maps(inputs)
    res = run_bass_kernel_spmd(nc, maps, core_ids=list(range(8)))
    out = np.stack([np.asarray(r["out"], np.float32) for r in res.results], axis=0)
    return out
```

```python
import numpy as np
import ml_dtypes
import concourse.bass as bass
import concourse.mybir as mybir
from concourse.bass_utils import run_bass_kernel_spmd

F32 = mybir.dt.float32
BF16 = mybir.dt.bfloat16
AF = mybir.ActivationFunctionType
ALU = mybir.AluOpType
AX = mybir.AxisListType

L = 2048
D = 2048
NB = 16
DIN = 9296
NIT = 14
TOPK = 256
MASKV = -60000.0
EPS = 1e-6
NEG = -1.0e30

ENGS = ["pe", "act", "dve", "pool", "sp"]
NDMASEM = 24
SELF_SYNC = True
UNITS = list(range(16))
STOP_UI = 0


class Tok:
    __slots__ = ("eng", "idx", "needed", "value", "semkey")

    def __init__(self, eng, idx):
        self.eng = eng
        self.idx = idx
        self.needed = False
        self.value = None
        self.semkey = eng


class Sched:
    def __init__(self):
        self.ops = {e: [] for e in ENGS}
        self.res = {}
        self.dma_n = 0
        self.dma_last = {}

    def _collect(self, eng, reads, writes, waits):
        ws = list(waits)
        for k in reads:
            r = self.res.get(k)
            if r is not None and r[0] is not None:
                ws.append(r[0])
        for k in writes:
            r = self.res.get(k)
            if r is not None:
                if r[0] is not None:
                    ws.append(r[0])
                ws.extend(r[1].values())
        best = {}
        for w in ws:
            if w is None:
                continue
            if w.eng == "sp":
                best[("d", w.semkey, w.value)] = w
                continue
            if w.eng == eng and (eng == "pe" or not SELF_SYNC):
                continue
            b = best.get(w.eng)
            if b is None or w.idx > b.idx:
                best[w.eng] = w
        out = list(best.values())
        for w in out:
            w.needed = True
        return out

    def op(self, eng, fn, reads=(), writes=(), waits=()):
        if eng != "pe":
            psr = [k for k in reads if isinstance(k, tuple) and k[0] == "ps"]
            if psr:
                reads = [k for k in reads if k not in psr]
                writes = list(writes) + [k for k in psr if k not in writes]
        ws = self._collect(eng, reads, writes, waits)
        tok = Tok(eng, len(self.ops[eng]))
        for k in reads:
            r = self.res.setdefault(k, [None, {}])
            r[1][eng] = tok
        for k in writes:
            self.res[k] = [tok, {}]
        self.ops[eng].append((fn, ws, tok))
        return tok

    def dma(self, fn, reads=(), writes=(), waits=()):
        k = self.dma_n % NDMASEM
        m = self.dma_n // NDMASEM + 1
        self.dma_n += 1
        ws = list(waits)
        prev = self.dma_last.get(k)
        if prev is not None:
            ws.append(prev)
        wl = self._collect("sp", reads, writes, ws)
        tok = Tok("sp", len(self.ops["sp"]))
        tok.semkey = ("dma", k)
        tok.value = 16 * m
        tok.needed = True
        self.dma_last[k] = tok
        for kk in reads:
            r = self.res.setdefault(kk, [None, {}])
            r[1]["sp"] = tok
        for kk in writes:
            self.res[kk] = [tok, {}]
        self.ops["sp"].append((fn, wl, tok))
        return tok

    def barrier(self):
        last = {}
        for e in ENGS:
            for (fn, ws, tok) in reversed(self.ops[e]):
                if fn is not None:
                    last[e] = tok
                    break
        if "sp" in last:
            sp_toks = list(self.dma_last.values())
        else:
            sp_toks = []
        for e in ENGS:
            ws = [t for (ee, t) in last.items() if ee != e and ee != "sp"] + (sp_toks if e != "sp" else [])
            for w in ws:
                w.needed = True
            self.ops[e].append((None, ws, None))

    def emit(self, nc, block, sems, dsems):
        for e in ENGS:
            if e == "sp":
                continue
            c = 0
            for (fn, ws, tok) in self.ops[e]:
                if tok is not None and tok.needed:
                    c += 1
                    tok.value = c

        def semof(w):
            if w.eng == "sp":
                return dsems[w.semkey[1]]
            return sems[w.eng]

        def run(ename):
            def body(eng):
                waited = {}
                for (fn, ws, tok) in self.ops[ename]:
                    for w in ws:
                        key = w.semkey
                        if waited.get(key, 0) >= w.value:
                            continue
                        eng.wait_ge(semof(w), w.value)
                        waited[key] = w.value
                    if fn is None:
                        continue
                    ins = fn(eng)
                    if ename == "sp":
                        ins.then_inc(semof(tok), 16)
                    elif tok.needed:
                        ins.then_inc(sems[ename], 1)
            return body

        block.tensor(run("pe"))
        block.scalar(run("act"))
        block.vector(run("dve"))
        block.gpsimd(run("pool"))
        block.sync(run("sp"))


def chunks(n, w=512):
    return [(c0, min(w, n - c0)) for c0 in range(0, n, w)]


def moff(tb):
    return 128 * (tb * (tb + 1) // 2)


def mtoff(i):
    return 2048 * i - 64 * i * (i - 1)


def build_nc(stop_after=None, dbg=None):
    nc = bass.Bass("TRN2", target_bir_lowering=False)
    dram = lambda n, s, dt=F32, kind="ExternalInput": nc.dram_tensor(n, s, dt, kind=kind).ap()
    x_d = dram("x", [L, D])
    c2_d = dram("c2", [128, 16])
    wada_d = dram("w_ada", [D, 3 * D])
    bsh_d = dram("bsh", [128, 16])
    bsc_d = dram("bsc", [128, 16])
    bgate_d = dram("bgate", [1, D])
    g2_d = dram("g2", [128, 16])
    win_d = dram("w_in", [D, DIN])
    gains_d = dram("gains", [128, 5])
    wout_d = dram("w_out", [D, D])
    identf_d = dram("identf", [128, 128])
    pow2_d = dram("pow2", [128, 32])
    biasc_d = dram("biasc", [128, 152])
    cbf_d = dram("cbf", [128, 1664], BF16)
    out_d = dram("out", [L, D], kind="ExternalOutput")
    dbg_d = None
    if dbg is not None:
        dbg_d = dram("dbg", list(dbg[1]), dbg[2], kind="ExternalOutput")

    S = Sched()
    from contextlib import ExitStack
    es = ExitStack()
    sb = lambda n, s, dt: es.enter_context(nc.sbuf_tensor(n, s, dt))
    with es:
        hT_t = sb("hT", [128, 16 * 2048], BF16)
        R1 = sb("R1", [128, 33792], BF16)
        RP = sb("RP", [128, 8448], BF16)
        stg_t = [sb("stg%d" % i, [128, 16 * 128], F32) for i in range(2)]
        wq_t = sb("wq", [128, 16 * 128], BF16)
        wk_t = sb("wk", [128, 16 * 128], BF16)
        wvz_t = sb("wvz", [128, 16 * 256], BF16)
        RT = sb("RT", [128, 2560], BF16)
        sqt_t = sb("sqt", [128, 512], BF16)
        sqt2_t = sb("sqt2", [128, 512], BF16)
        sd_t = sb("sd", [128, 512], F32)
        bmT_t = sb("bmT", [128, 2048], BF16)
        gatebc = sb("gatebc", [128, D], F32)
        identf = sb("identf_s", [128, 128], F32)
        cbf = sb("cbf_s", [128, 1664], BF16)
        pow2 = sb("pow2_s", [128, 32], F32)
        small = sb("small", [128, 640], F32)
        onesf = sb("onesf", [128, 128], F32)

        ps_t = [es.enter_context(nc.psum_tensor("ps%d" % i, [128, 512], F32)) for i in range(8)]
        PS = [p[:, :] for p in ps_t]

        hT = hT_t[:, :].rearrange("p (a t) -> p a t", a=16)
        qiT = R1[:, 0:16384].rearrange("p (a t) -> p a t", a=8)
        maskv = R1[:, 16384:33792]
        yT = R1[:, 0:32768].rearrange("p (a t) -> p a t", a=16)
        stgA = [R1[:, 0:8192].bitcast(F32).rearrange("p (a n) -> p a n", a=16),
                R1[:, 8192:16384].bitcast(F32).rearrange("p (a n) -> p a n", a=16)]
        modrow = R1[:, 16384:16384 + 12288].bitcast(F32)
        xt = [RP[:, 0:4096].bitcast(F32), RP[:, 4096:8192].bitcast(F32)]
        acc = xt
        qT = RP[:, 0:2048]
        kT = RP[:, 2048:4096]
        szT = RP[:, 4096:6144]
        Vv = RP[:, 6144:8192].rearrange("p (a d) -> p a d", a=16)
        stg = [t[:, :].rearrange("p (a n) -> p a n", a=16) for t in stg_t]
        wq = wq_t[:, :].rearrange("p (a n) -> p a n", a=16)
        wk = wk_t[:, :].rearrange("p (a n) -> p a n", a=16)
        wvz = wvz_t[:, :].rearrange("p (a n) -> p a n", a=16)
        wv = wvz_t[:, 0:2048].rearrange("p (a n) -> p a n", a=16)
        wz = wvz_t[:, 2048:4096].rearrange("p (a n) -> p a n", a=16)
        junk = wvz_t[:, 0:2048]
        mtok = wvz_t[:, 2048:4096]
        junk8 = wvz_t[:, 0:2048].bitcast(mybir.dt.uint8)
        kiT = RT[:, 0:2048]
        wtok = RT[:, 2048:2560].bitcast(F32).rearrange("p (a h) -> p a h", a=16)
        Pt = [RT[:, i * 512:(i + 1) * 512] for i in range(4)]
        ytok = [RT[:, 2048:2176], RT[:, 2176:2304]]
        sqt = sqt_t[:, :]
        sqt2 = sqt2_t[:, :]
        sd = sd_t[:, :]
        bmT = bmT_t[:, :]
        ident_bf = cbf[:, 0:128]
        ones_bf = cbf[:, 128:256]
        blockones = cbf[:, 256:384]
        ctri = cbf[:, 384:512]
        atab = cbf[:, 512:640]
        btab = cbf[:, 640:1152]
        zeros_bf = cbf[:, 1152:1664]
        sc = small[:, 0:16]
        Avec = small[:, 16:32]
        Shv = small[:, 32:48]
        bsh = small[:, 48:64]
        bsc = small[:, 64:80]
        g2 = small[:, 80:96]
        gains = small[:, 96:101]
        gq = small[:, 104:120]
        ss = small[:, 120:121]
        std = small[:, 121:122]
        rstd = small[:, 122:123]
        rmax = small[:, 123:124]
        rmin = small[:, 124:125]
        w0 = small[:, 125:126]
        mid = small[:, 126:127]
        cnt = small[:, 127:128]
        uu = small[:, 128:129]
        thr = small[:, 129:130]
        steps = small[:, 136:168]
        bm = small[:, 168:296].rearrange("p (a n) -> p a n", a=16)
        gt = small[:, 296:304]
        m8 = small[:, 304:312]
        ksum = small[:, 312:320]
        rden = small[:, 320:336]
        c2 = small[:, 336:352]
        epsc = small[:, 368:369]
        biasc = small[:, 400:552]
        tmp16 = small[:, 352:368]
        ksb = cbf_ks = sb("ksb", [128, 8], BF16)[:, :]
        bgrow = R1[0:1, 28672:28672 + 4096].bitcast(F32)

        S.dma(lambda e: e.dma_start(out=identf[:, :], in_=identf_d), writes=["identf"])
        S.dma(lambda e: e.dma_start(out=cbf[:, :], in_=cbf_d), writes=["cbf"])
        S.dma(lambda e: e.dma_start(out=pow2[:, :], in_=pow2_d), writes=["pow2"])
        S.dma(lambda e: e.dma_start(out=biasc, in_=biasc_d), writes=["biasc"])
        S.dma(lambda e: e.dma_start(out=c2, in_=c2_d), writes=["c2"])
        S.dma(lambda e: e.dma_start(out=bsh, in_=bsh_d), writes=["bsh"])
        S.dma(lambda e: e.dma_start(out=bsc, in_=bsc_d), writes=["bsc"])
        S.dma(lambda e: e.dma_start(out=g2, in_=g2_d), writes=["g2"])
        S.dma(lambda e: e.dma_start(out=gains, in_=gains_d), writes=["gains"])
        S.dma(lambda e: e.dma_start(out=bgrow, in_=bgate_d), writes=["bgrow"])
        S.op("pool", lambda e: e.memset(onesf[:, :], 1.0), writes=["onesf"])
        S.op("pool", lambda e: e.memset(epsc, EPS), writes=["epsc"])

        S.op("act", lambda e: e.activation(out=sc, in_=c2, func=AF.Silu), reads=["c2"], writes=["sc"])
        if stop_after == "A1":
            return finish(nc, S, es, dbg, dbg_d, locals())
        wada_v = wada_d.rearrange("(kh kl) n -> kl kh n", kl=128)
        NA = 24
        for ci in range(NA):
            sl = ci % 2
            c0 = ci * 256
            S.dma(lambda e, sl=sl, c0=c0: e.dma_start(out=stgA[sl], in_=wada_v[:, :, c0:c0 + 256]),
                  writes=[("stgA", sl)])
            pb = ci % 2
            for kh in range(16):
                S.op("pe", lambda e, sl=sl, kh=kh, pb=pb: e.matmul(
                    out=PS[pb][0:1, 0:256], lhsT=sc[:, kh:kh + 1], rhs=stgA[sl][:, kh, :],
                    start=(kh == 0), stop=(kh == 15)),
                    reads=[("stgA", sl), "sc"], writes=[("ps", pb)])
            S.op("dve", lambda e, c0=c0, pb=pb: e.tensor_copy(out=modrow[0:1, c0:c0 + 256], in_=PS[pb][0:1, 0:256]),
                 reads=[("ps", pb)], writes=["modrow"])
        if stop_after == "A2":
            return finish(nc, S, es, dbg, dbg_d, locals())
        for j in range(32):
            S.op("pe", lambda e, j=j: e.matmul(out=PS[2][:, j:j + 1], lhsT=modrow[0:1, j * 128:(j + 1) * 128],
                                               rhs=identf[0:1, 0:1], start=True, stop=True),
                 reads=["modrow", "identf"], writes=[("ps", 2)])
        S.op("dve", lambda e: e.tensor_tensor(out=Shv, in0=PS[2][:, 0:16], in1=bsh, op=ALU.add),
             reads=[("ps", 2), "bsh"], writes=["Shv"])
        S.op("dve", lambda e: e.tensor_tensor(out=tmp16, in0=PS[2][:, 16:32], in1=bsc, op=ALU.add),
             reads=[("ps", 2), "bsc"], writes=["tmp16"])
        S.op("dve", lambda e: e.scalar_tensor_tensor(out=Avec, in0=tmp16, scalar=1.0, in1=g2, op0=ALU.add, op1=ALU.mult),
             reads=["tmp16", "g2"], writes=["Avec"])
        if stop_after == "A3":
            return finish(nc, S, es, dbg, dbg_d, locals())
        S.op("dve", lambda e: e.tensor_tensor(out=modrow[0:1, 4096:6144], in0=modrow[0:1, 4096:6144], in1=bgrow,
                                              op=ALU.add), reads=["modrow", "bgrow"], writes=["modrow"])
        for q in range(4):
            pb = 3 + (q % 2)
            S.op("pe", lambda e, q=q, pb=pb: e.matmul(out=PS[pb][:, :], lhsT=onesf[0:1, 0:128],
                                                      rhs=modrow[0:1, 4096 + q * 512:4096 + (q + 1) * 512],
                                                      start=True, stop=True),
                 reads=["modrow", "onesf"], writes=[("ps", pb)])
            S.op("act", lambda e, q=q, pb=pb: e.activation(out=gatebc[:, q * 512:(q + 1) * 512], in_=PS[pb][:, :],
                                                           func=AF.Identity),
                 reads=[("ps", pb)], writes=["gatebc"])
        for u in range(16):
            h = u % 8
            col = 0 if u < 8 else 3
            fac = (128.0 ** -0.5) * (2.0 ** (h + 1))
            S.op("dve", lambda e, u=u, col=col, fac=fac: e.tensor_scalar(
                out=gq[:, u:u + 1], in0=gains[:, col:col + 1], scalar1=fac, scalar2=None, op0=ALU.mult),
                reads=["gains"], writes=["gq"])
        S.barrier()
        if stop_after == "A":
            return finish(nc, S, es, dbg, dbg_d, locals())

        for tb in range(NB):
            sl = tb % 2
            S.dma(lambda e, sl=sl, tb=tb: e.dma_start(out=xt[sl], in_=x_d[tb * 128:(tb + 1) * 128, :]),
                  writes=[("xt", sl)])
            S.op("act", lambda e, sl=sl: e.activation(out=junk, in_=xt[sl], func=AF.Square, accum_out=ss),
                 reads=[("xt", sl)], writes=["junk", "ss"])
            S.op("act", lambda e: e.activation(out=std, in_=ss, func=AF.Sqrt, bias=EPS, scale=1.0 / D),
                 reads=["ss"], writes=["std"])
            S.op("dve", lambda e: e.reciprocal(out=rstd, in_=std), reads=["std"], writes=["rstd"])
            S.op("dve", lambda e, sl=sl: e.tensor_scalar(out=xt[sl], in0=xt[sl], scalar1=rstd, scalar2=None,
                                                         op0=ALU.mult),
                 reads=["rstd", ("xt", sl)], writes=[("xt", sl)])
            if stop_after == "B1":
                return finish(nc, S, es, dbg, dbg_d, locals())
            for g in range(4):
                pb = g
                if stop_after == "B3" and g == 1:
                    return finish(nc, S, es, dbg, dbg_d, locals())
                for qd in range(4):
                    dh = g * 4 + qd
                    S.op("pe", lambda e, sl=sl, dh=dh, pb=pb, qd=qd: e.transpose(
                        out=PS[pb][:, qd * 128:(qd + 1) * 128], in_=xt[sl][:, dh * 128:(dh + 1) * 128],
                        identity=identf[:, :]),
                        reads=[("xt", sl), "identf"], writes=[("ps", pb)])
                if stop_after == "B2":
                    return finish(nc, S, es, dbg, dbg_d, locals())
                for qd in range(4):
                    dh = g * 4 + qd
                    if qd % 2 == 0:
                        S.op("act", lambda e, tb=tb, dh=dh, pb=pb, qd=qd: e.activation(
                            out=hT[:, dh, tb * 128:(tb + 1) * 128], in_=PS[pb][:, qd * 128:(qd + 1) * 128],
                            func=AF.Identity, bias=Shv[:, dh:dh + 1], scale=Avec[:, dh:dh + 1]),
                            reads=[("ps", pb), "Shv", "Avec"], writes=[("hT", tb)])
                    else:
                        S.op("dve", lambda e, tb=tb, dh=dh, pb=pb, qd=qd: e.tensor_scalar(
                            out=hT[:, dh, tb * 128:(tb + 1) * 128], in0=PS[pb][:, qd * 128:(qd + 1) * 128],
                            scalar1=Avec[:, dh:dh + 1], scalar2=Shv[:, dh:dh + 1], op0=ALU.mult, op1=ALU.add),
                            reads=[("ps", pb), "Shv", "Avec"], writes=[("hT", tb)])
        S.barrier()
        if stop_after == "B":
            return finish(nc, S, es, dbg, dbg_d, locals())

        win_v = win_d.rearrange("(dh dl) n -> dl dh n", dl=128)
        state = {"stg": 0, "ps": 0, "sbank": 0, "pslot": 0}

        def load_slab(c0, ncol, casts):
            sl = state["stg"] % 2
            state["stg"] += 1
            S.dma(lambda e, sl=sl: e.dma_start(out=stg[sl][:, :, 0:ncol], in_=win_v[:, :, c0:c0 + ncol]),
                  writes=[("stg", sl)])
            for (dst, s0, s1, key) in casts:
                S.op("pool", lambda e, sl=sl, dst=dst, s0=s0, s1=s1: e.tensor_copy(out=dst, in_=stg[sl][:, :, s0:s1]),
                     reads=[("stg", sl)], writes=[key])

        def next_ps(n=2, base=0):
            b = base + state["ps"] % n
            state["ps"] += 1
            return b

        def proj_fm(wslab, wkey, tc, pb):
            for dh in range(16):
                S.op("pe", lambda e, dh=dh: e.matmul(out=PS[pb][:, :], lhsT=wslab[:, dh, :],
                                                     rhs=hT[:, dh, tc * 512:(tc + 1) * 512],
                                                     start=(dh == 0), stop=(dh == 15)),
                     reads=[wkey] + [("hT", tc * 4 + i) for i in range(4)], writes=[("ps", pb)])

        sqs = [sqt, sqt2]

        def norm_a(pb, qi_):
            S.op("act", lambda e: e.activation(out=sqs[qi_], in_=PS[pb][:, :], func=AF.Square),
                 reads=[("ps", pb)], writes=[("sqt", qi_)])

        def norm_b(pb, pb2, qi_, onesmat, ndim, gcol, dst, dkey):
            S.op("pe", lambda e: e.matmul(out=PS[pb2][:, :], lhsT=onesmat, rhs=sqs[qi_], start=True, stop=True),
                 reads=[("sqt", qi_), "cbf"], writes=[("ps", pb2)])
            S.op("act", lambda e: e.activation(out=sd, in_=PS[pb2][:, :], func=AF.Ln, bias=epsc, scale=1.0 / ndim),
                 reads=[("ps", pb2), "epsc"], writes=["sd"])
            S.op("act", lambda e: e.activation(out=sd, in_=sd, func=AF.Exp, scale=-0.5),
                 reads=["sd"], writes=["sd"])
            S.op("dve", lambda e: e.scalar_tensor_tensor(out=dst, in0=PS[pb][:, :], scalar=gcol, in1=sd,
                                                         op0=ALU.mult, op1=ALU.mult),
                 reads=[("ps", pb), "sd", "gains", "gq"], writes=[dkey])

        def norm_evac(pb, pb2, onesmat, ndim, gcol, dst, dkey):
            norm_a(pb, 0)
            norm_b(pb, pb2, 0, onesmat, ndim, gcol, dst, dkey)

        def proj_norm_pipe(jobs):
            prev = None
            for n_, (w_, wk_, tc, gcol, dst, dkey) in enumerate(jobs):
                pb = n_ % 3
                proj_fm(w_, wk_, tc, pb)
                norm_a(pb, n_ % 2)
                if prev is not None:
                    norm_b(*prev)
                prev = (pb, 3, n_ % 2, ones_bf, 128.0, gcol, dst, dkey)
            norm_b(*prev)

        for p in range(8):
            load_slab(4096 + 128 * p, 128, [(wq, 0, 128, "wq")])
            for tc in range(4):
                pb = next_ps(2, 0)
                proj_fm(wq, "wq", tc, pb)
                S.op("act", lambda e, p=p, tc=tc, pb=pb: e.activation(
                    out=qiT[:, p, tc * 512:(tc + 1) * 512], in_=PS[pb][:, :], func=AF.Identity),
                    reads=[("ps", pb)], writes=[("qiT", p)])
        load_slab(5120, 80, [(wk[:, :, 0:64], 0, 64, "wk"), (wk[:, :, 64:128], 0, 64, "wk"),
                             (wvz[:, :, 0:16], 64, 80, "wvz")])
        for tc in range(4):
            pb = next_ps(2, 0)
            proj_fm(wk, "wk", tc, pb)
            norm_evac(pb, 2 + tc % 2, blockones, 64.0, gains[:, 2:3], kiT[:, tc * 512:(tc + 1) * 512], ("kiT", tc))
        for tb in range(NB):
            pb = 4 + tb % 2
            for dh in range(16):
                S.op("pe", lambda e, tb=tb, dh=dh, pb=pb: e.matmul(
                    out=PS[pb][:, 0:16], lhsT=hT[:, dh, tb * 128:(tb + 1) * 128], rhs=wvz[:, dh, 0:16],
                    start=(dh == 0), stop=(dh == 15)),
                    reads=["wvz", ("hT", tb)], writes=[("ps", pb)])
            S.op("dve", lambda e, tb=tb, pb=pb: e.tensor_copy(out=wtok[:, tb, :], in_=PS[pb][:, 0:16]),
                 reads=[("ps", pb)], writes=[("wtok", tb)])
        S.barrier()
        if stop_after == "C":
            return finish(nc, S, es, dbg, dbg_d, locals())

        psr = 0
        kiT_hi = wq_t[:, 0:2048]
        kkeys = [("kiT", i) for i in range(4)]
        S.op("pool", lambda e: e.tensor_copy(out=kiT_hi[64:128, :], in_=kiT[64:128, :]), reads=kkeys, writes=["kiThi"])
        S.op("pool", lambda e: e.memset(kiT_hi[0:64, :], 0.0), writes=["kiThi"])
        S.op("pool", lambda e: e.memset(kiT[64:128, :], 0.0), reads=["kiThi"], writes=kkeys)
        dgs = [bmT_t[:, :].rearrange("p (h n) -> p h n", h=16), wk_t[:, :].rearrange("p (h n) -> p h n", h=16)]
        sd_bf = sd_t[:, :].bitcast(BF16)
        rts = [sqt, sqt2, sd_bf[:, 0:512], sd_bf[:, 512:1024]]
        dstate = {"lb": 0, "rs": 0, "sc": 0}
        accs = [xt[0], xt[1], stg_t[0][:, :], stg_t[1][:, :]]
        scr = [dict(rmax=rmax, rmin=rmin, w0=w0, mid=mid, cnt=cnt, uu=uu, thr=thr, steps=steps[:, 0:NIT + 1], tag="0"),
               dict(rmax=small[:, 369:370], rmin=small[:, 370:371], w0=small[:, 371:372], mid=small[:, 372:373],
                    cnt=small[:, 373:374], uu=small[:, 374:375], thr=small[:, 375:376], steps=small[:, 376:376 + NIT + 1],
                    tag="1")]

        def build_dg(tb):
            dg = dgs[tb % 2]
            S.op("dve", lambda e: e.tensor_tensor(
                out=dg, in0=ident_bf.unsqueeze(1).to_broadcast([128, 16, 128]),
                in1=wtok[:, tb, :].unsqueeze(2).to_broadcast([128, 16, 128]), op=ALU.mult),
                reads=[("wtok", tb), "cbf"], writes=[("dg", tb % 2)])

        def do_scores(tb, a, akey):
            ncol = 128 * (tb + 1)
            dg = dgs[tb % 2]
            dgk = ("dg", tb % 2)
            for (c0, cw) in chunks(ncol):
                scb = 4 + dstate["sc"] % 2
                dstate["sc"] += 1

                def emit_L(h, c0=c0, cw=cw, tb=tb):
                    pb = dstate["lb"] % 4
                    dstate["lb"] += 1
                    ksrc = kiT if h % 2 == 0 else kiT_hi
                    kk = kkeys if h % 2 == 0 else ["kiThi"]
                    S.op("pe", lambda e: e.matmul(
                        out=PS[pb][:, 0:cw], lhsT=qiT[:, h // 2, tb * 128:(tb + 1) * 128], rhs=ksrc[:, c0:c0 + cw],
                        start=True, stop=True),
                        reads=[("qiT", h // 2)] + kk, writes=[("ps", pb)])
                    return pb

                def emit_relu(h, pb, cw=cw):
                    rs_ = dstate["rs"] % 4
                    dstate["rs"] += 1
                    S.op("act", lambda e: e.activation(out=rts[rs_][:, 0:cw], in_=PS[pb][:, 0:cw], func=AF.Relu),
                         reads=[("ps", pb)], writes=[("rt", rs_)])
                    return rs_

                def emit_acc(h, rs_, cw=cw, scb=scb, dg=dg, dgk=dgk):
                    S.op("pe", lambda e: e.matmul(
                        out=PS[scb][:, 0:cw], lhsT=dg[:, h, :], rhs=rts[rs_][:, 0:cw], start=(h == 0), stop=(h == 15)),
                        reads=[("rt", rs_), dgk], writes=[("ps", scb)])

                LA = 2
                pbs = {}
                for h in range(LA):
                    pbs[h] = emit_L(h)
                for h in range(16):
                    if h + LA < 16:
                        pbs[h + LA] = emit_L(h + LA)
                    rs_ = emit_relu(h, pbs[h])
                    emit_acc(h, rs_)
                S.op("act", lambda e, a=a, c0=c0, cw=cw, scb=scb: e.activation(out=a[:, c0:c0 + cw], in_=PS[scb][:, 0:cw],
                                                                               func=AF.Identity),
                     reads=[("ps", scb)], writes=[akey])

        def prep(tb, a, akey, sc):
            ncol = 128 * (tb + 1)
            t = sc["tag"]
            if tb >= 2:
                S.op("dve", lambda e: e.tensor_reduce(out=sc["rmax"], in_=a[:, 0:ncol], axis=AX.X, op=ALU.max),
                     reads=[akey], writes=["rmax" + t])
                S.op("dve", lambda e: e.tensor_reduce(out=sc["rmin"], in_=a[:, 0:ncol], axis=AX.X, op=ALU.min),
                     reads=[akey], writes=["rmin" + t])
            S.op("pool", lambda e: e.affine_select(
                out=a[:, tb * 128:(tb + 1) * 128], in_=a[:, tb * 128:(tb + 1) * 128], pattern=[[-1, 128]],
                compare_op=ALU.is_ge, fill=NEG, base=0, channel_multiplier=1),
                reads=[akey], writes=[akey])
            if tb >= 2:
                S.op("dve", lambda e: e.tensor_tensor(out=sc["w0"], in0=sc["rmax"], in1=sc["rmin"], op=ALU.subtract),
                     reads=["rmax" + t, "rmin" + t], writes=["w0" + t])
                S.op("dve", lambda e: e.tensor_scalar(out=sc["steps"], in0=pow2[:, 0:NIT + 1], scalar1=sc["w0"], scalar2=None,
                                                      op0=ALU.mult),
                     reads=["w0" + t, "pow2"], writes=["steps" + t])
                S.op("dve", lambda e: e.tensor_tensor(out=sc["mid"], in0=sc["rmin"], in1=sc["steps"][:, 0:1], op=ALU.add),
                     reads=["rmin" + t, "steps" + t], writes=["mid" + t])
            else:
                S.op("dve", lambda e: e.memset(sc["thr"], -1.0e29), writes=["thr" + t])

        def it_pass(tb, a, akey, sc, k):
            ncol = 128 * (tb + 1)
            t = sc["tag"]
            jk = junk8[:, 0:ncol] if t == "0" else junk8[:, 2048:2048 + ncol]
            S.op("dve", lambda e: e.tensor_scalar(
                out=jk, in0=a[:, 0:ncol], scalar1=sc["mid"], scalar2=None, op0=ALU.is_ge, op1=ALU.add,
                accum_out=sc["cnt"]),
                reads=[akey, "mid" + t], writes=["cnt" + t, "junk" + t])

        def it_u(tb, a, akey, sc, k):
            t = sc["tag"]
            S.op("dve", lambda e: e.tensor_scalar(out=sc["uu"], in0=sc["cnt"], scalar1=TOPK - 0.5, scalar2=0.5,
                                                  op0=ALU.is_gt, op1=ALU.subtract),
                 reads=["cnt" + t], writes=["uu" + t])

        def it_mid(tb, a, akey, sc, k):
            t = sc["tag"]
            S.op("dve", lambda e: e.scalar_tensor_tensor(out=sc["mid"], in0=sc["uu"], scalar=sc["steps"][:, k:k + 1],
                                                         in1=sc["mid"], op0=ALU.mult, op1=ALU.add),
                 reads=["uu" + t, "steps" + t, "mid" + t], writes=["mid" + t])

        def fin(tb, a, akey, sc):
            nonlocal psr
            ncol = 128 * (tb + 1)
            t = sc["tag"]
            if tb >= 2:
                S.op("dve", lambda e: e.tensor_tensor(out=sc["thr"], in0=sc["mid"], in1=sc["steps"][:, NIT:NIT + 1],
                                                      op=ALU.subtract),
                     reads=["mid" + t, "steps" + t], writes=["thr" + t])
            S.op("dve", lambda e: e.tensor_scalar(
                out=mtok[:, 0:ncol], in0=a[:, 0:ncol], scalar1=sc["thr"], scalar2=MASKV, op0=ALU.is_lt, op1=ALU.mult),
                reads=[akey, "thr" + t], writes=["mtok"])
            for g0 in range(0, tb + 1, 4):
                pb = 6 + psr % 2
                psr += 1
                blk = list(range(g0, min(g0 + 4, tb + 1)))
                for qi_, i in enumerate(blk):
                    S.op("pe", lambda e, qi_=qi_, i=i, pb=pb: e.transpose(
                        out=PS[pb][:, qi_ * 64:(qi_ + 1) * 64].bitcast(BF16), in_=mtok[:, i * 128:(i + 1) * 128],
                        identity=ident_bf),
                        reads=["mtok", "cbf"], writes=[("ps", pb)])
                for qi_, i in enumerate(blk):
                    mo = mtoff(i) + (tb - i) * 128
                    S.op("act", lambda e, qi_=qi_, mo=mo, pb=pb: e.activation(
                        out=maskv[:, mo:mo + 128], in_=PS[pb][:, qi_ * 64:(qi_ + 1) * 64].bitcast(BF16), func=AF.Identity),
                        reads=[("ps", pb)], writes=[("maskT", i)])

        for pi in range(8):
            items = []
            for q_ in range(2):
                tb = 2 * pi + q_
                ai = (2 * pi + q_) % 4
                items.append((tb, accs[ai], ("acc", ai), scr[q_]))
            if pi == 0:
                build_dg(0)
                build_dg(1)
            for it in items:
                do_scores(it[0], it[1], it[2])
            if pi < 7:
                build_dg(2 * pi + 2)
                build_dg(2 * pi + 3)
            for it in items:
                prep(*it)
            if pi >= 1:
                for k in range(NIT):
                    for fnk in (it_pass, it_u, it_mid):
                        for it in items:
                            fnk(*it, k)
            for it in items:
                fin(*it)
        S.barrier()
        if stop_after == "D":
            return finish(nc, S, es, dbg, dbg_d, locals())

        S.op("pool", lambda e: e.memset(bmT, 0.0), writes=["bmT"])
        QB = {0: (0, 1024, 2048, 3072), 1: (5200, 6224, 7248, 8272)}
        mbt = small[:, 560:624].bitcast(BF16)

        def load_unit(u):
            grp, h = u // 8, u % 8
            qb, kb, vb, zb = QB[grp]
            load_slab(qb + 128 * h, 128, [(wq, 0, 128, "wq")])
            load_slab(kb + 128 * h, 128, [(wk, 0, 128, "wk")])
            load_slab(vb + 128 * h, 128, [(wv, 0, 128, "wv")])
            load_slab(zb + 128 * h, 128, [(wz, 0, 128, "wz")])

        load_unit(UNITS[0])
        for ui, u in enumerate(UNITS):
            grp, h = u // 8, u % 8
            slope = 2.0 ** (-(h + 1))
            kcol = 1 if grp == 0 else 4
            proj_norm_pipe([(wq, "wq", tc, gq[:, u:u + 1], qT[:, tc * 512:(tc + 1) * 512], ("qT", tc)) for tc in range(4)] +
                           [(wk, "wk", tc, gains[:, kcol:kcol + 1], kT[:, tc * 512:(tc + 1) * 512], ("kT", tc))
                            for tc in range(4)])
            if stop_after == "E1" and ui == STOP_UI:
                return finish(nc, S, es, dbg, dbg_d, locals())
            if grp == 1:
                S.op("dve", lambda e: e.tensor_reduce(out=ksum, in_=kT.rearrange("p (n s) -> p n s", n=8), axis=AX.X,
                                                      op=ALU.add),
                     reads=[("kT", i) for i in range(4)], writes=["ksum"])
                S.op("dve", lambda e: e.tensor_copy(out=ksb, in_=ksum), reads=["ksum"], writes=["ksb"])
                for tb in range(NB):
                    S.op("pe", lambda e, tb=tb: e.matmul(out=PS[7][:, tb * 8:(tb + 1) * 8], lhsT=qT[:, tb * 128:(tb + 1) * 128],
                                                         rhs=ksb, start=True, stop=True),
                         reads=[("qT", tb // 4), "ksb"], writes=[("ps", 7)])
                for tb in range(NB):
                    n = tb // 2
                    S.op("dve", lambda e, tb=tb: e.tensor_copy(out=gt, in_=PS[7][:, tb * 8:(tb + 1) * 8]),
                         reads=[("ps", 7)], writes=["gt"])
                    if n < 8:
                        S.op("dve", lambda e, n=n: e.memset(gt[:, n:8], NEG), reads=["gt"], writes=["gt"])
                    S.op("dve", lambda e: e.max(out=m8, in_=gt), reads=["gt"], writes=["m8"])
                    S.op("dve", lambda e, tb=tb: e.tensor_scalar(out=mbt[:, tb * 8:(tb + 1) * 8], in0=gt, scalar1=m8[:, 2:3],
                                                                 scalar2=MASKV, op0=ALU.is_lt, op1=ALU.mult),
                         reads=["gt", "m8"], writes=["mbt"])
                for g in range(2):
                    pb = next_ps(2, 0)
                    for q_ in range(8):
                        tb = 8 * g + q_
                        S.op("pe", lambda e, tb=tb, q_=q_, pb=pb: e.transpose(
                            out=PS[pb][0:8, q_ * 64:(q_ + 1) * 64].bitcast(BF16), in_=mbt[:, tb * 8:(tb + 1) * 8],
                            identity=ident_bf),
                            reads=["mbt", "cbf"], writes=[("ps", pb)])
                    S.op("act", lambda e, g=g, pb=pb: e.activation(
                        out=bmT[0:8, g * 1024:(g + 1) * 1024], in_=PS[pb][0:8, :].bitcast(BF16), func=AF.Identity),
                        reads=[("ps", pb)], writes=["bmT"])
            for g in range(4):
                pb = next_ps(2, 0)
                for q_ in range(4):
                    tb = 4 * g + q_
                    for dh in range(16):
                        S.op("pe", lambda e, tb=tb, dh=dh, pb=pb, q_=q_: e.matmul(
                            out=PS[pb][:, q_ * 128:(q_ + 1) * 128], lhsT=hT[:, dh, tb * 128:(tb + 1) * 128],
                            rhs=wv[:, dh, :], start=(dh == 0), stop=(dh == 15)),
                            reads=["wv", ("hT", tb)], writes=[("ps", pb)])
                S.op("dve", lambda e, g=g, pb=pb: e.tensor_copy(
                    out=RP[:, 6144 + g * 512:6144 + (g + 1) * 512], in_=PS[pb][:, :]),
                    reads=[("ps", pb)], writes=[("V", g)])
            for tc in range(4):
                pb = next_ps(2, 0)
                proj_fm(wz, "wz", tc, pb)
                S.op("act", lambda e, tc=tc, pb=pb: e.activation(out=szT[:, tc * 512:(tc + 1) * 512], in_=PS[pb][:, :],
                                                                 func=AF.Silu),
                     reads=[("ps", pb)], writes=[("szT", tc)])
            if stop_after == "E2" and ui == STOP_UI:
                return finish(nc, S, es, dbg, dbg_d, locals())
            if ui + 1 < len(UNITS):
                load_unit(UNITS[ui + 1])
            pslot = 0
            sbank = 0
            for j in range(4):
                tb0 = 4 * j
                o1 = 4 + 2 * (j % 2)
                o2 = 5 + 2 * (j % 2)
                ni = 4 * j + 4
                def emit_S(i):
                    nonlocal_state = state
                    tlo = max(tb0, i)
                    c0 = (tlo - tb0) * 128
                    sb_ = state["sbank"] % 4
                    state["sbank"] += 1
                    nprime = i // 2
                    mm = []
                    mm.append((kT[:, i * 128:(i + 1) * 128], qT[:, tlo * 128:(tb0 + 4) * 128], c0, 512,
                               [("kT", i // 4), ("qT", j)]))
                    mm.append((atab, btab[:, c0:512], c0, 512, ["cbf"]))
                    if grp == 0:
                        mo = mtoff(i) + (tlo - i) * 128
                        mm.append((ident_bf, maskv[:, mo:mo + 512 - c0], c0, 512, [("maskT", i), "cbf"]))
                    else:
                        gl = max(tlo, 2 * nprime + 2)
                        if gl < tb0 + 4:
                            gc = (gl - tb0) * 128
                            mm.append((ident_bf[:, nprime:nprime + 1].to_broadcast([128, 128]),
                                       bmT[:, gl * 128:(tb0 + 4) * 128], gc, 512, ["bmT", "cbf"]))
                        if i >= tb0:
                            dc_ = (i - tb0) * 128
                            mm.append((ident_bf, ctri, dc_, dc_ + 128, ["cbf"]))
                    for mi, (lh, rh, a_, b_, rk) in enumerate(mm):
                        S.op("pe", lambda e, lh=lh, rh=rh, a_=a_, b_=b_, sb_=sb_, mi=mi, last=(mi == len(mm) - 1): e.matmul(
                            out=PS[sb_][:, a_:b_], lhsT=lh, rhs=rh, start=(mi == 0), stop=last),
                            reads=rk, writes=[("ps", sb_)])
                    return sb_, c0

                def emit_exp(i, sb_, c0):
                    ps_ = state["pslot"] % 4
                    state["pslot"] += 1
                    bcol = h * 19 + (i - tb0 + 15)
                    S.op("act", lambda e, bcol=bcol, slope=slope: e.activation(
                        out=Pt[ps_][:, c0:512], in_=PS[sb_][:, c0:512], func=AF.Exp,
                        bias=biasc[:, bcol:bcol + 1], scale=slope),
                        reads=[("ps", sb_), "biasc"], writes=[("P", ps_)])
                    return ps_

                def emit_PV(i, ps_, c0):
                    S.op("pe", lambda e, o1=o1, ni=ni: e.matmul(
                        out=PS[o1][:, c0:512], lhsT=Vv[:, i, :], rhs=Pt[ps_][:, c0:512], start=(i == 0), stop=(i == ni - 1)),
                        reads=[("P", ps_), ("V", i // 4)], writes=[("ps", o1)])
                    S.op("pe", lambda e, o2=o2, ni=ni: e.matmul(
                        out=PS[o2][:, c0:512], lhsT=ones_bf, rhs=Pt[ps_][:, c0:512], start=(i == 0), stop=(i == ni - 1)),
                        reads=[("P", ps_), "cbf"], writes=[("ps", o2)])

                LA = 2
                info = {}
                for i in range(min(LA, ni)):
                    info[i] = emit_S(i)
                for i in range(ni):
                    if i + LA < ni:
                        info[i + LA] = emit_S(i + LA)
                    ps_ = emit_exp(i, *info[i])
                    emit_PV(i, ps_, info[i][1])
                if stop_after == "E3" and ui == STOP_UI:
                    return finish(nc, S, es, dbg, dbg_d, locals())
                S.op("act", lambda e, o2=o2: e.activation(out=sd, in_=PS[o2][:, :], func=AF.Ln), reads=[("ps", o2)],
                     writes=["sd"])
                S.op("act", lambda e: e.activation(out=sd, in_=sd, func=AF.Exp, scale=-1.0), reads=["sd"], writes=["sd"])
                S.op("dve", lambda e, j=j: e.tensor_tensor(out=sd, in0=sd, in1=szT[:, j * 512:(j + 1) * 512], op=ALU.mult),
                     reads=["sd", ("szT", j)], writes=["sd"])
                S.op("dve", lambda e, u=u, j=j, o1=o1: e.tensor_tensor(
                    out=yT[:, u, j * 512:(j + 1) * 512], in0=PS[o1][:, :], in1=sd, op=ALU.mult),
                    reads=[("ps", o1), "sd"], writes=[("yT", 4 * j), ("yT", 4 * j + 1), ("yT", 4 * j + 2), ("yT", 4 * j + 3)])
            if stop_after == "E4" and ui == STOP_UI:
                return finish(nc, S, es, dbg, dbg_d, locals())
            if u == 7:
                S.barrier()
        S.barrier()
        if stop_after == "E":
            return finish(nc, S, es, dbg, dbg_d, locals())

        wout_v = wout_d.rearrange("(mh ml) n -> ml mh n", ml=128)
        wob = hT
        for cc in range(16):
            sl = state["stg"] % 2
            state["stg"] += 1
            c0 = cc * 128
            S.dma(lambda e, sl=sl, c0=c0: e.dma_start(out=stg[sl], in_=wout_v[:, :, c0:c0 + 128]), writes=[("stg", sl)])
            S.op("act", lambda e, sl=sl, c0=c0: e.activation(out=wob[:, :, c0:c0 + 128], in_=stg[sl], func=AF.Identity),
                 reads=[("stg", sl)], writes=[("wob", cc // 4)])
        xo = xt[0]
        ot = xt[1]
        out_toks = []
        for tb in range(NB):
            S.dma(lambda e, tb=tb: e.dma_start(out=xo, in_=x_d[tb * 128:(tb + 1) * 128, :]), writes=["xo"])
            for dc in range(4):
                pb = next_ps(4, 0)
                for mh in range(16):
                    S.op("pe", lambda e, tb=tb, dc=dc, mh=mh, pb=pb: e.matmul(
                        out=PS[pb][:, :], lhsT=yT[:, mh, tb * 128:(tb + 1) * 128], rhs=wob[:, mh, dc * 512:(dc + 1) * 512],
                        start=(mh == 0), stop=(mh == 15)),
                        reads=[("yT", tb), ("wob", dc)], writes=[("ps", pb)])
                S.op("dve", lambda e, dc=dc, pb=pb: e.tensor_tensor(
                    out=ot[:, dc * 512:(dc + 1) * 512], in0=PS[pb][:, :], in1=gatebc[:, dc * 512:(dc + 1) * 512], op=ALU.mult),
                    reads=[("ps", pb), "gatebc"], writes=["ot"])
                S.op("dve", lambda e, dc=dc: e.tensor_tensor(
                    out=ot[:, dc * 512:(dc + 1) * 512], in0=ot[:, dc * 512:(dc + 1) * 512],
                    in1=xo[:, dc * 512:(dc + 1) * 512], op=ALU.add),
                    reads=["ot", "xo"], writes=["ot"])
            out_toks.append(S.dma(lambda e, tb=tb: e.dma_start(out=out_d[tb * 128:(tb + 1) * 128, :], in_=ot),
                                  reads=["ot"], writes=[("outd", tb)]))
        S.ops["sp"].append((None, out_toks, None))
        return finish(nc, S, es, None, None, locals())


def finish(nc, S, es, dbg, dbg_d, env):
    if dbg is not None:
        src = env[dbg[0]]
        S.barrier()
        t = S.dma(lambda e: e.dma_start(out=dbg_d, in_=dbg[3](env)), reads=[], writes=["dbgout"])
        S.ops["sp"].append((None, [t], None))
    from contextlib import ExitStack
    with ExitStack() as es2:
        sems = {e: es2.enter_context(nc.semaphore("sem_" + e)) for e in ENGS if e != "sp"}
        dsems = [es2.enter_context(nc.semaphore("dsem%d" % i)) for i in range(NDMASEM)]
        block = es2.enter_context(nc.Block())
        S.emit(nc, block, sems, dsems)
    return nc


def _consts():
    identf = np.eye(128, dtype=np.float32)
    pow2 = np.tile((2.0 ** -(np.arange(32) + 1.0)).astype(np.float32)[None, :], (128, 1))
    cb = np.zeros((128, 1664), dtype=np.float32)
    cb[:, 0:128] = np.eye(128)
    cb[:, 128:256] = 1.0
    bo = np.zeros((128, 128), np.float32)
    bo[:64, :64] = 1.0
    bo[64:, 64:] = 1.0
    cb[:, 256:384] = bo
    sidx = np.arange(128)[:, None]
    tidx = np.arange(128)[None, :]
    cb[:, 384:512] = np.where(sidx > tidx, MASKV, 0.0)
    at = np.zeros((128, 128), np.float32)
    at[0, :] = np.arange(128)
    at[1, :] = 1.0
    at[2, :] = 1.0
    cb[:, 512:640] = at
    bt = np.zeros((128, 512), np.float32)
    tr = np.arange(512)
    bt[0, :] = 1.0
    bt[1, :] = -128.0 * (tr // 128)
    bt[2, :] = -(tr % 128)
    cb[:, 640:1152] = bt
    bc = np.zeros((128, 152), np.float32)
    for h in range(8):
        for v in range(-15, 4):
            bc[:, h * 19 + v + 15] = (2.0 ** -(h + 1)) * 128.0 * v
    return identf, pow2, cb.astype(ml_dtypes.bfloat16), bc


def _in_maps(inputs):
    x = np.asarray(inputs["x"], np.float32)
    c = np.asarray(inputs["c"], np.float32)
    w_ada = np.ascontiguousarray(np.asarray(inputs["w_ada"], np.float32)[0])
    b_ada = np.asarray(inputs["b_ada"], np.float32)[0]
    g_norm = np.asarray(inputs["g_norm"], np.float32)[0]
    w_in = np.ascontiguousarray(np.asarray(inputs["w_in"], np.float32)[0])
    w_out = np.ascontiguousarray(np.asarray(inputs["w_out"], np.float32)[0])
    lay = lambda v: np.ascontiguousarray(v.reshape(16, 128).T)
    kni = np.asarray(inputs["k_norm_idx"], np.float32)[0]
    gains = np.stack([np.asarray(inputs["q_norm_a"], np.float32)[0], np.asarray(inputs["k_norm_a"], np.float32)[0],
                      np.concatenate([kni, kni]), np.asarray(inputs["q_norm_b"], np.float32)[0],
                      np.asarray(inputs["k_norm_b"], np.float32)[0]], axis=1).astype(np.float32)
    identf, pow2, cbf, biasc = _consts()
    maps = []
    for b in range(8):
        maps.append({
            "x": np.ascontiguousarray(x[b]), "c2": lay(c[b]), "w_ada": w_ada,
            "bsh": lay(b_ada[0:D]), "bsc": lay(b_ada[D:2 * D]), "bgate": np.ascontiguousarray(b_ada[2 * D:3 * D][None, :]),
            "g2": lay(g_norm), "w_in": w_in, "gains": np.ascontiguousarray(gains), "w_out": w_out,
            "identf": identf, "pow2": pow2, "cbf": cbf, "biasc": biasc,
        })
    return maps


_NC_CACHE = {}


def kernel(**inputs):
    if "nc" not in _NC_CACHE:
        _NC_CACHE["nc"] = build_nc()
    nc = _NC_CACHE["nc"]
    maps = _in_maps(inputs)
    res = run_bass_kernel_spmd(nc, maps, core_ids=list(range(8)))
    out = np.stack([np.asarray(r["out"], np.float32) for r in res.results], axis=0)
    return out
```

```python
import numpy as np
import ml_dtypes
import concourse.bass as bass
import concourse.mybir as mybir
from concourse.bass_utils import run_bass_kernel_spmd

F32 = mybir.dt.float32
BF16 = mybir.dt.bfloat16
AF = mybir.ActivationFunctionType
ALU = mybir.AluOpType
AX = mybir.AxisListType

L = 2048
D = 2048
NB = 16
DIN = 9296
NIT = 14
TOPK = 256
MASKV = -60000.0
EPS = 1e-6
NEG = -1.0e30

ENGS = ["pe", "act", "dve", "pool", "sp"]
NDMASEM = 24
SELF_SYNC = True
UNITS = list(range(16))
STOP_UI = 0


class Tok:
    __slots__ = ("eng", "idx", "needed", "value", "semkey")

    def __init__(self, eng, idx):
        self.eng = eng
        self.idx = idx
        self.needed = False
        self.value = None
        self.semkey = eng


class Sched:
    def __init__(self):
        self.ops = {e: [] for e in ENGS}
        self.res = {}
        self.dma_n = 0
        self.dma_last = {}

    def _collect(self, eng, reads, writes, waits):
        ws = list(waits)
        for k in reads:
            r = self.res.get(k)
            if r is not None and r[0] is not None:
                ws.append(r[0])
        for k in writes:
            r = self.res.get(k)
            if r is not None:
                if r[0] is not None:
                    ws.append(r[0])
                ws.extend(r[1].values())
        best = {}
        for w in ws:
            if w is None:
                continue
            if w.eng == "sp":
                best[("d", w.semkey, w.value)] = w
                continue
            if w.eng == eng and (eng == "pe" or not SELF_SYNC):
                continue
            b = best.get(w.eng)
            if b is None or w.idx > b.idx:
                best[w.eng] = w
        out = list(best.values())
        for w in out:
            w.needed = True
        return out

    def op(self, eng, fn, reads=(), writes=(), waits=()):
        if eng != "pe":
            psr = [k for k in reads if isinstance(k, tuple) and k[0] == "ps"]
            if psr:
                reads = [k for k in reads if k not in psr]
                writes = list(writes) + [k for k in psr if k not in writes]
        ws = self._collect(eng, reads, writes, waits)
        tok = Tok(eng, len(self.ops[eng]))
        for k in reads:
            r = self.res.setdefault(k, [None, {}])
            r[1][eng] = tok
        for k in writes:
            self.res[k] = [tok, {}]
        self.ops[eng].append((fn, ws, tok))
        return tok

    def dma(self, fn, reads=(), writes=(), waits=()):
        k = self.dma_n % NDMASEM
        m = self.dma_n // NDMASEM + 1
        self.dma_n += 1
        ws = list(waits)
        prev = self.dma_last.get(k)
        if prev is not None:
            ws.append(prev)
        wl = self._collect("sp", reads, writes, ws)
        tok = Tok("sp", len(self.ops["sp"]))
        tok.semkey = ("dma", k)
        tok.value = 16 * m
        tok.needed = True
        self.dma_last[k] = tok
        for kk in reads:
            r = self.res.setdefault(kk, [None, {}])
            r[1]["sp"] = tok
        for kk in writes:
            self.res[kk] = [tok, {}]
        self.ops["sp"].append((fn, wl, tok))
        return tok

    def barrier(self):
        last = {}
        for e in ENGS:
            for (fn, ws, tok) in reversed(self.ops[e]):
                if fn is not None:
                    last[e] = tok
                    break
        if "sp" in last:
            sp_toks = list(self.dma_last.values())
        else:
            sp_toks = []
        for e in ENGS:
            ws = [t for (ee, t) in last.items() if ee != e and ee != "sp"] + (sp_toks if e != "sp" else [])
            for w in ws:
                w.needed = True
            self.ops[e].append((None, ws, None))

    def emit(self, nc, block, sems, dsems):
        for e in ENGS:
            if e == "sp":
                continue
            c = 0
            for (fn, ws, tok) in self.ops[e]:
                if tok is not None and tok.needed:
                    c += 1
                    tok.value = c

        def semof(w):
            if w.eng == "sp":
                return dsems[w.semkey[1]]
            return sems[w.eng]

        def run(ename):
            def body(eng):
                waited = {}
                for (fn, ws, tok) in self.ops[ename]:
                    for w in ws:
                        key = w.semkey
                        if waited.get(key, 0) >= w.value:
                            continue
                        eng.wait_ge(semof(w), w.value)
                        waited[key] = w.value
                    if fn is None:
                        continue
                    ins = fn(eng)
                    if ename == "sp":
                        ins.then_inc(semof(tok), 16)
                    elif tok.needed:
                        ins.then_inc(sems[ename], 1)
            return body

        block.tensor(run("pe"))
        block.scalar(run("act"))
        block.vector(run("dve"))
        block.gpsimd(run("pool"))
        block.sync(run("sp"))


def chunks(n, w=512):
    return [(c0, min(w, n - c0)) for c0 in range(0, n, w)]


def moff(tb):
    return 128 * (tb * (tb + 1) // 2)


def mtoff(i):
    return 2048 * i - 64 * i * (i - 1)


def build_nc(stop_after=None, dbg=None):
    nc = bass.Bass("TRN2", target_bir_lowering=False)
    dram = lambda n, s, dt=F32, kind="ExternalInput": nc.dram_tensor(n, s, dt, kind=kind).ap()
    x_d = dram("x", [L, D])
    c2_d = dram("c2", [128, 16])
    wada_d = dram("w_ada", [D, 3 * D])
    bsh_d = dram("bsh", [128, 16])
    bsc_d = dram("bsc", [128, 16])
    bgate_d = dram("bgate", [1, D])
    g2_d = dram("g2", [128, 16])
    win_d = dram("w_in", [D, DIN])
    gains_d = dram("gains", [128, 5])
    wout_d = dram("w_out", [D, D])
    identf_d = dram("identf", [128, 128])
    pow2_d = dram("pow2", [128, 32])
    biasc_d = dram("biasc", [128, 152])
    cbf_d = dram("cbf", [128, 1664], BF16)
    out_d = dram("out", [L, D], kind="ExternalOutput")
    dbg_d = None
    if dbg is not None:
        dbg_d = dram("dbg", list(dbg[1]), dbg[2], kind="ExternalOutput")

    S = Sched()
    from contextlib import ExitStack
    es = ExitStack()
    sb = lambda n, s, dt: es.enter_context(nc.sbuf_tensor(n, s, dt))
    with es:
        hT_t = sb("hT", [128, 16 * 2048], BF16)
        R1 = sb("R1", [128, 33792], BF16)
        RP = sb("RP", [128, 8448], BF16)
        stg_t = [sb("stg%d" % i, [128, 16 * 128], F32) for i in range(2)]
        wq_t = sb("wq", [128, 16 * 128], BF16)
        wk_t = sb("wk", [128, 16 * 128], BF16)
        wvz_t = sb("wvz", [128, 16 * 256], BF16)
        RT = sb("RT", [128, 2560], BF16)
        sqt_t = sb("sqt", [128, 512], BF16)
        sqt2_t = sb("sqt2", [128, 512], BF16)
        sd_t = sb("sd", [128, 512], F32)
        bmT_t = sb("bmT", [128, 2048], BF16)
        gatebc = sb("gatebc", [128, D], F32)
        identf = sb("identf_s", [128, 128], F32)
        cbf = sb("cbf_s", [128, 1664], BF16)
        pow2 = sb("pow2_s", [128, 32], F32)
        small = sb("small", [128, 640], F32)
        onesf = sb("onesf", [128, 128], F32)

        ps_t = [es.enter_context(nc.psum_tensor("ps%d" % i, [128, 512], F32)) for i in range(8)]
        PS = [p[:, :] for p in ps_t]

        hT = hT_t[:, :].rearrange("p (a t) -> p a t", a=16)
        qiT = R1[:, 0:16384].rearrange("p (a t) -> p a t", a=8)
        maskv = R1[:, 16384:33792]
        yT = R1[:, 0:32768].rearrange("p (a t) -> p a t", a=16)
        stgA = [R1[:, 0:8192].bitcast(F32).rearrange("p (a n) -> p a n", a=16),
                R1[:, 8192:16384].bitcast(F32).rearrange("p (a n) -> p a n", a=16)]
        modrow = R1[:, 16384:16384 + 12288].bitcast(F32)
        xt = [RP[:, 0:4096].bitcast(F32), RP[:, 4096:8192].bitcast(F32)]
        acc = xt
        qT = RP[:, 0:2048]
        kT = RP[:, 2048:4096]
        szT = RP[:, 4096:6144]
        Vv = RP[:, 6144:8192].rearrange("p (a d) -> p a d", a=16)
        stg = [t[:, :].rearrange("p (a n) -> p a n", a=16) for t in stg_t]
        wq = wq_t[:, :].rearrange("p (a n) -> p a n", a=16)
        wk = wk_t[:, :].rearrange("p (a n) -> p a n", a=16)
        wvz = wvz_t[:, :].rearrange("p (a n) -> p a n", a=16)
        wv = wvz_t[:, 0:2048].rearrange("p (a n) -> p a n", a=16)
        wz = wvz_t[:, 2048:4096].rearrange("p (a n) -> p a n", a=16)
        junk = wvz_t[:, 0:2048]
        mtok = wvz_t[:, 2048:4096]
        junk8 = wvz_t[:, 0:2048].bitcast(mybir.dt.uint8)
        kiT = RT[:, 0:2048]
        wtok = RT[:, 2048:2560].bitcast(F32).rearrange("p (a h) -> p a h", a=16)
        Pt = [RT[:, i * 512:(i + 1) * 512] for i in range(4)]
        ytok = [RT[:, 2048:2176], RT[:, 2176:2304]]
        sqt = sqt_t[:, :]
        sqt2 = sqt2_t[:, :]
        sd = sd_t[:, :]
        bmT = bmT_t[:, :]
        ident_bf = cbf[:, 0:128]
        ones_bf = cbf[:, 128:256]
        blockones = cbf[:, 256:384]
        ctri = cbf[:, 384:512]
        atab = cbf[:, 512:640]
        btab = cbf[:, 640:1152]
        zeros_bf = cbf[:, 1152:1664]
        sc = small[:, 0:16]
        Avec = small[:, 16:32]
        Shv = small[:, 32:48]
        bsh = small[:, 48:64]
        bsc = small[:, 64:80]
        g2 = small[:, 80:96]
        gains = small[:, 96:101]
        gq = small[:, 104:120]
        ss = small[:, 120:121]
        std = small[:, 121:122]
        rstd = small[:, 122:123]
        rmax = small[:, 123:124]
        rmin = small[:, 124:125]
        w0 = small[:, 125:126]
        mid = small[:, 126:127]
        cnt = small[:, 127:128]
        uu = small[:, 128:129]
        thr = small[:, 129:130]
        steps = small[:, 136:168]
        bm = small[:, 168:296].rearrange("p (a n) -> p a n", a=16)
        gt = small[:, 296:304]
        m8 = small[:, 304:312]
        ksum = small[:, 312:320]
        rden = small[:, 320:336]
        c2 = small[:, 336:352]
        epsc = small[:, 368:369]
        biasc = small[:, 400:552]
        tmp16 = small[:, 352:368]
        ksb = cbf_ks = sb("ksb", [128, 8], BF16)[:, :]
        bgrow = R1[0:1, 28672:28672 + 4096].bitcast(F32)

        S.dma(lambda e: e.dma_start(out=identf[:, :], in_=identf_d), writes=["identf"])
        S.dma(lambda e: e.dma_start(out=cbf[:, :], in_=cbf_d), writes=["cbf"])
        S.dma(lambda e: e.dma_start(out=pow2[:, :], in_=pow2_d), writes=["pow2"])
        S.dma(lambda e: e.dma_start(out=biasc, in_=biasc_d), writes=["biasc"])
        S.dma(lambda e: e.dma_start(out=c2, in_=c2_d), writes=["c2"])
        S.dma(lambda e: e.dma_start(out=bsh, in_=bsh_d), writes=["bsh"])
        S.dma(lambda e: e.dma_start(out=bsc, in_=bsc_d), writes=["bsc"])
        S.dma(lambda e: e.dma_start(out=g2, in_=g2_d), writes=["g2"])
        S.dma(lambda e: e.dma_start(out=gains, in_=gains_d), writes=["gains"])
        S.dma(lambda e: e.dma_start(out=bgrow, in_=bgate_d), writes=["bgrow"])
        S.op("pool", lambda e: e.memset(onesf[:, :], 1.0), writes=["onesf"])
        S.op("pool", lambda e: e.memset(epsc, EPS), writes=["epsc"])

        S.op("act", lambda e: e.activation(out=sc, in_=c2, func=AF.Silu), reads=["c2"], writes=["sc"])
        if stop_after == "A1":
            return finish(nc, S, es, dbg, dbg_d, locals())
        wada_v = wada_d.rearrange("(kh kl) n -> kl kh n", kl=128)
        def ada_chunk(ci):
            sl = ci % 2
            c0 = ci * 256
            S.dma(lambda e, sl=sl, c0=c0: e.dma_start(out=stgA[sl], in_=wada_v[:, :, c0:c0 + 256]),
                  writes=[("stgA", sl)])
            pb = ci % 2
            for kh in range(16):
                S.op("pe", lambda e, sl=sl, kh=kh, pb=pb: e.matmul(
                    out=PS[pb][0:1, 0:256], lhsT=sc[:, kh:kh + 1], rhs=stgA[sl][:, kh, :],
                    start=(kh == 0), stop=(kh == 15)),
                    reads=[("stgA", sl), "sc"], writes=[("ps", pb)])
            S.op("dve", lambda e, c0=c0, pb=pb: e.tensor_copy(out=modrow[0:1, c0:c0 + 256], in_=PS[pb][0:1, 0:256]),
                 reads=[("ps", pb)], writes=["modrow"])

        for ci in range(16):
            ada_chunk(ci)
        if stop_after == "A2":
            return finish(nc, S, es, dbg, dbg_d, locals())
        for j in range(32):
            S.op("pe", lambda e, j=j: e.matmul(out=PS[2][:, j:j + 1], lhsT=modrow[0:1, j * 128:(j + 1) * 128],
                                               rhs=identf[0:1, 0:1], start=True, stop=True),
                 reads=["modrow", "identf"], writes=[("ps", 2)])
        S.op("dve", lambda e: e.tensor_tensor(out=Shv, in0=PS[2][:, 0:16], in1=bsh, op=ALU.add),
             reads=[("ps", 2), "bsh"], writes=["Shv"])
        S.op("dve", lambda e: e.tensor_tensor(out=tmp16, in0=PS[2][:, 16:32], in1=bsc, op=ALU.add),
             reads=[("ps", 2), "bsc"], writes=["tmp16"])
        S.op("dve", lambda e: e.scalar_tensor_tensor(out=Avec, in0=tmp16, scalar=1.0, in1=g2, op0=ALU.add, op1=ALU.mult),
             reads=["tmp16", "g2"], writes=["Avec"])
        if stop_after == "A3":
            return finish(nc, S, es, dbg, dbg_d, locals())
        for u in range(16):
            h = u % 8
            col = 0 if u < 8 else 3
            fac = (128.0 ** -0.5) * (2.0 ** (h + 1))
            S.op("dve", lambda e, u=u, col=col, fac=fac: e.tensor_scalar(
                out=gq[:, u:u + 1], in0=gains[:, col:col + 1], scalar1=fac, scalar2=None, op0=ALU.mult),
                reads=["gains"], writes=["gq"])
        S.barrier()
        if stop_after == "A":
            return finish(nc, S, es, dbg, dbg_d, locals())

        for tb in range(NB):
            sl = tb % 2
            S.dma(lambda e, sl=sl, tb=tb: e.dma_start(out=xt[sl], in_=x_d[tb * 128:(tb + 1) * 128, :]),
                  writes=[("xt", sl)])
            if tb % 2 == 1:
                ada_chunk(16 + tb // 2)
            S.op("act", lambda e, sl=sl: e.activation(out=junk, in_=xt[sl], func=AF.Square, accum_out=ss),
                 reads=[("xt", sl)], writes=["junk", "ss"])
            S.op("act", lambda e: e.activation(out=std, in_=ss, func=AF.Sqrt, bias=EPS, scale=1.0 / D),
                 reads=["ss"], writes=["std"])
            S.op("dve", lambda e: e.reciprocal(out=rstd, in_=std), reads=["std"], writes=["rstd"])
            S.op("dve", lambda e, sl=sl: e.tensor_scalar(out=xt[sl], in0=xt[sl], scalar1=rstd, scalar2=None,
                                                         op0=ALU.mult),
                 reads=["rstd", ("xt", sl)], writes=[("xt", sl)])
            if stop_after == "B1":
                return finish(nc, S, es, dbg, dbg_d, locals())
            for g in range(4):
                pb = g
                if stop_after == "B3" and g == 1:
                    return finish(nc, S, es, dbg, dbg_d, locals())
                for qd in range(4):
                    dh = g * 4 + qd
                    S.op("pe", lambda e, sl=sl, dh=dh, pb=pb, qd=qd: e.transpose(
                        out=PS[pb][:, qd * 128:(qd + 1) * 128], in_=xt[sl][:, dh * 128:(dh + 1) * 128],
                        identity=identf[:, :]),
                        reads=[("xt", sl), "identf"], writes=[("ps", pb)])
                if stop_after == "B2":
                    return finish(nc, S, es, dbg, dbg_d, locals())
                for qd in range(4):
                    dh = g * 4 + qd
                    if qd % 2 == 0:
                        S.op("act", lambda e, tb=tb, dh=dh, pb=pb, qd=qd: e.activation(
                            out=hT[:, dh, tb * 128:(tb + 1) * 128], in_=PS[pb][:, qd * 128:(qd + 1) * 128],
                            func=AF.Identity, bias=Shv[:, dh:dh + 1], scale=Avec[:, dh:dh + 1]),
                            reads=[("ps", pb), "Shv", "Avec"], writes=[("hT", tb)])
                    else:
                        S.op("dve", lambda e, tb=tb, dh=dh, pb=pb, qd=qd: e.tensor_scalar(
                            out=hT[:, dh, tb * 128:(tb + 1) * 128], in0=PS[pb][:, qd * 128:(qd + 1) * 128],
                            scalar1=Avec[:, dh:dh + 1], scalar2=Shv[:, dh:dh + 1], op0=ALU.mult, op1=ALU.add),
                            reads=[("ps", pb), "Shv", "Avec"], writes=[("hT", tb)])
        S.op("dve", lambda e: e.tensor_tensor(out=modrow[0:1, 4096:6144], in0=modrow[0:1, 4096:6144], in1=bgrow,
                                              op=ALU.add), reads=["modrow", "bgrow"], writes=["modrow"])
        for q in range(4):
            pb = 3 + (q % 2)
            S.op("pe", lambda e, q=q, pb=pb: e.matmul(out=PS[pb][:, :], lhsT=onesf[0:1, 0:128],
                                                      rhs=modrow[0:1, 4096 + q * 512:4096 + (q + 1) * 512],
                                                      start=True, stop=True),
                 reads=["modrow", "onesf"], writes=[("ps", pb)])
            S.op("act", lambda e, q=q, pb=pb: e.activation(out=gatebc[:, q * 512:(q + 1) * 512], in_=PS[pb][:, :],
                                                           func=AF.Identity),
                 reads=[("ps", pb)], writes=["gatebc"])
        S.barrier()
        if stop_after == "B":
            return finish(nc, S, es, dbg, dbg_d, locals())

        win_v = win_d.rearrange("(dh dl) n -> dl dh n", dl=128)
        state = {"stg": 0, "ps": 0, "sbank": 0, "pslot": 0}

        def load_slab(c0, ncol, casts):
            sl = state["stg"] % 2
            state["stg"] += 1
            S.dma(lambda e, sl=sl: e.dma_start(out=stg[sl][:, :, 0:ncol], in_=win_v[:, :, c0:c0 + ncol]),
                  writes=[("stg", sl)])
            for (dst, s0, s1, key) in casts:
                S.op("pool", lambda e, sl=sl, dst=dst, s0=s0, s1=s1: e.tensor_copy(out=dst, in_=stg[sl][:, :, s0:s1]),
                     reads=[("stg", sl)], writes=[key])

        def next_ps(n=2, base=0):
            b = base + state["ps"] % n
            state["ps"] += 1
            return b

        def proj_fm(wslab, wkey, tc, pb):
            for dh in range(16):
                S.op("pe", lambda e, dh=dh: e.matmul(out=PS[pb][:, :], lhsT=wslab[:, dh, :],
                                                     rhs=hT[:, dh, tc * 512:(tc + 1) * 512],
                                                     start=(dh == 0), stop=(dh == 15)),
                     reads=[wkey] + [("hT", tc * 4 + i) for i in range(4)], writes=[("ps", pb)])

        sqs = [sqt, sqt2]

        def norm_a(pb, qi_):
            S.op("act", lambda e: e.activation(out=sqs[qi_], in_=PS[pb][:, :], func=AF.Square),
                 reads=[("ps", pb)], writes=[("sqt", qi_)])

        def norm_b(pb, pb2, qi_, onesmat, ndim, gcol, dst, dkey):
            S.op("pe", lambda e: e.matmul(out=PS[pb2][:, :], lhsT=onesmat, rhs=sqs[qi_], start=True, stop=True),
                 reads=[("sqt", qi_), "cbf"], writes=[("ps", pb2)])
            S.op("act", lambda e: e.activation(out=sd, in_=PS[pb2][:, :], func=AF.Ln, bias=epsc, scale=1.0 / ndim),
                 reads=[("ps", pb2), "epsc"], writes=["sd"])
            S.op("act", lambda e: e.activation(out=sd, in_=sd, func=AF.Exp, scale=-0.5),
                 reads=["sd"], writes=["sd"])
            S.op("dve", lambda e: e.scalar_tensor_tensor(out=dst, in0=PS[pb][:, :], scalar=gcol, in1=sd,
                                                         op0=ALU.mult, op1=ALU.mult),
                 reads=[("ps", pb), "sd", "gains", "gq"], writes=[dkey])

        def norm_evac(pb, pb2, onesmat, ndim, gcol, dst, dkey):
            norm_a(pb, 0)
            norm_b(pb, pb2, 0, onesmat, ndim, gcol, dst, dkey)

        def proj_norm_pipe(jobs):
            prev = None
            for n_, (w_, wk_, tc, gcol, dst, dkey) in enumerate(jobs):
                pb = n_ % 3
                proj_fm(w_, wk_, tc, pb)
                norm_a(pb, n_ % 2)
                if prev is not None:
                    norm_b(*prev)
                prev = (pb, 3, n_ % 2, ones_bf, 128.0, gcol, dst, dkey)
            norm_b(*prev)

        for p in range(8):
            load_slab(4096 + 128 * p, 128, [(wq, 0, 128, "wq")])
            for tc in range(4):
                pb = next_ps(2, 0)
                proj_fm(wq, "wq", tc, pb)
                S.op("act", lambda e, p=p, tc=tc, pb=pb: e.activation(
                    out=qiT[:, p, tc * 512:(tc + 1) * 512], in_=PS[pb][:, :], func=AF.Identity),
                    reads=[("ps", pb)], writes=[("qiT", p)])
        load_slab(5120, 80, [(wk[:, :, 0:64], 0, 64, "wk"), (wk[:, :, 64:128], 0, 64, "wk"),
                             (wvz[:, :, 0:16], 64, 80, "wvz")])
        for tc in range(4):
            pb = next_ps(2, 0)
            proj_fm(wk, "wk", tc, pb)
            norm_evac(pb, 2 + tc % 2, blockones, 64.0, gains[:, 2:3], kiT[:, tc * 512:(tc + 1) * 512], ("kiT", tc))
        for tb in range(NB):
            pb = 4 + tb % 2
            for dh in range(16):
                S.op("pe", lambda e, tb=tb, dh=dh, pb=pb: e.matmul(
                    out=PS[pb][:, 0:16], lhsT=hT[:, dh, tb * 128:(tb + 1) * 128], rhs=wvz[:, dh, 0:16],
                    start=(dh == 0), stop=(dh == 15)),
                    reads=["wvz", ("hT", tb)], writes=[("ps", pb)])
            S.op("dve", lambda e, tb=tb, pb=pb: e.tensor_copy(out=wtok[:, tb, :], in_=PS[pb][:, 0:16]),
                 reads=[("ps", pb)], writes=[("wtok", tb)])
        S.barrier()
        if stop_after == "C":
            return finish(nc, S, es, dbg, dbg_d, locals())

        psr = 0
        kiT_hi = wq_t[:, 0:2048]
        kkeys = [("kiT", i) for i in range(4)]
        S.op("pool", lambda e: e.tensor_copy(out=kiT_hi[64:128, :], in_=kiT[64:128, :]), reads=kkeys, writes=["kiThi"])
        S.op("pool", lambda e: e.memset(kiT_hi[0:64, :], 0.0), writes=["kiThi"])
        S.op("pool", lambda e: e.memset(kiT[64:128, :], 0.0), reads=["kiThi"], writes=kkeys)
        dgs = [bmT_t[:, :].rearrange("p (h n) -> p h n", h=16), wk_t[:, :].rearrange("p (h n) -> p h n", h=16)]
        sd_bf = sd_t[:, :].bitcast(BF16)
        rts = [sqt, sqt2, sd_bf[:, 0:512], sd_bf[:, 512:1024]]
        dstate = {"lb": 0, "rs": 0, "sc": 0}
        accs = [xt[0], xt[1], stg_t[0][:, :], stg_t[1][:, :]]
        scr = [dict(rmax=rmax, rmin=rmin, w0=w0, mid=mid, cnt=cnt, uu=uu, thr=thr, steps=steps[:, 0:NIT + 1], tag="0"),
               dict(rmax=small[:, 369:370], rmin=small[:, 370:371], w0=small[:, 371:372], mid=small[:, 372:373],
                    cnt=small[:, 373:374], uu=small[:, 374:375], thr=small[:, 375:376], steps=small[:, 376:376 + NIT + 1],
                    tag="1")]

        def build_dg(tb):
            dg = dgs[tb % 2]
            S.op("dve", lambda e: e.tensor_tensor(
                out=dg, in0=ident_bf.unsqueeze(1).to_broadcast([128, 16, 128]),
                in1=wtok[:, tb, :].unsqueeze(2).to_broadcast([128, 16, 128]), op=ALU.mult),
                reads=[("wtok", tb), "cbf"], writes=[("dg", tb % 2)])

        def do_scores(tb, a, akey):
            ncol = 128 * (tb + 1)
            dg = dgs[tb % 2]
            dgk = ("dg", tb % 2)
            for (c0, cw) in chunks(ncol):
                scb = 4 + dstate["sc"] % 2
                dstate["sc"] += 1

                def emit_L(h, c0=c0, cw=cw, tb=tb):
                    pb = dstate["lb"] % 4
                    dstate["lb"] += 1
                    ksrc = kiT if h % 2 == 0 else kiT_hi
                    kk = kkeys if h % 2 == 0 else ["kiThi"]
                    S.op("pe", lambda e: e.matmul(
                        out=PS[pb][:, 0:cw], lhsT=qiT[:, h // 2, tb * 128:(tb + 1) * 128], rhs=ksrc[:, c0:c0 + cw],
                        start=True, stop=True),
                        reads=[("qiT", h // 2)] + kk, writes=[("ps", pb)])
                    return pb

                def emit_relu(h, pb, cw=cw):
                    rs_ = dstate["rs"] % 4
                    dstate["rs"] += 1
                    S.op("act", lambda e: e.activation(out=rts[rs_][:, 0:cw], in_=PS[pb][:, 0:cw], func=AF.Relu),
                         reads=[("ps", pb)], writes=[("rt", rs_)])
                    return rs_

                def emit_acc(h, rs_, cw=cw, scb=scb, dg=dg, dgk=dgk):
                    S.op("pe", lambda e: e.matmul(
                        out=PS[scb][:, 0:cw], lhsT=dg[:, h, :], rhs=rts[rs_][:, 0:cw], start=(h == 0), stop=(h == 15)),
                        reads=[("rt", rs_), dgk], writes=[("ps", scb)])

                LA = 2
                pbs = {}
                for h in range(LA):
                    pbs[h] = emit_L(h)
                for h in range(16):
                    if h + LA < 16:
                        pbs[h + LA] = emit_L(h + LA)
                    rs_ = emit_relu(h, pbs[h])
                    emit_acc(h, rs_)
                S.op("act", lambda e, a=a, c0=c0, cw=cw, scb=scb: e.activation(out=a[:, c0:c0 + cw], in_=PS[scb][:, 0:cw],
                                                                               func=AF.Identity),
                     reads=[("ps", scb)], writes=[akey])

        def prep(tb, a, akey, sc):
            ncol = 128 * (tb + 1)
            t = sc["tag"]
            if tb >= 2:
                S.op("dve", lambda e: e.tensor_reduce(out=sc["rmax"], in_=a[:, 0:ncol], axis=AX.X, op=ALU.max),
                     reads=[akey], writes=["rmax" + t])
                S.op("dve", lambda e: e.tensor_reduce(out=sc["rmin"], in_=a[:, 0:ncol], axis=AX.X, op=ALU.min),
                     reads=[akey], writes=["rmin" + t])
            S.op("pool", lambda e: e.affine_select(
                out=a[:, tb * 128:(tb + 1) * 128], in_=a[:, tb * 128:(tb + 1) * 128], pattern=[[-1, 128]],
                compare_op=ALU.is_ge, fill=NEG, base=0, channel_multiplier=1),
                reads=[akey], writes=[akey])
            if tb >= 2:
                S.op("dve", lambda e: e.tensor_tensor(out=sc["w0"], in0=sc["rmax"], in1=sc["rmin"], op=ALU.subtract),
                     reads=["rmax" + t, "rmin" + t], writes=["w0" + t])
                S.op("dve", lambda e: e.tensor_scalar(out=sc["steps"], in0=pow2[:, 0:NIT + 1], scalar1=sc["w0"], scalar2=None,
                                                      op0=ALU.mult),
                     reads=["w0" + t, "pow2"], writes=["steps" + t])
                S.op("dve", lambda e: e.tensor_tensor(out=sc["mid"], in0=sc["rmin"], in1=sc["steps"][:, 0:1], op=ALU.add),
                     reads=["rmin" + t, "steps" + t], writes=["mid" + t])
            else:
                S.op("dve", lambda e: e.memset(sc["thr"], -1.0e29), writes=["thr" + t])

        def it_pass(tb, a, akey, sc, k):
            ncol = 128 * (tb + 1)
            t = sc["tag"]
            jk = junk8[:, 0:ncol] if t == "0" else junk8[:, 2048:2048 + ncol]
            S.op("dve", lambda e: e.tensor_scalar(
                out=jk, in0=a[:, 0:ncol], scalar1=sc["mid"], scalar2=None, op0=ALU.is_ge, op1=ALU.add,
                accum_out=sc["cnt"]),
                reads=[akey, "mid" + t], writes=["cnt" + t, "junk" + t])

        def it_u(tb, a, akey, sc, k):
            t = sc["tag"]
            S.op("dve", lambda e: e.tensor_scalar(out=sc["uu"], in0=sc["cnt"], scalar1=TOPK - 0.5, scalar2=0.5,
                                                  op0=ALU.is_gt, op1=ALU.subtract),
                 reads=["cnt" + t], writes=["uu" + t])

        def it_mid(tb, a, akey, sc, k):
            t = sc["tag"]
            S.op("dve", lambda e: e.scalar_tensor_tensor(out=sc["mid"], in0=sc["uu"], scalar=sc["steps"][:, k:k + 1],
                                                         in1=sc["mid"], op0=ALU.mult, op1=ALU.add),
                 reads=["uu" + t, "steps" + t, "mid" + t], writes=["mid" + t])

        def fin(tb, a, akey, sc):
            nonlocal psr
            ncol = 128 * (tb + 1)
            t = sc["tag"]
            if tb >= 2:
                S.op("dve", lambda e: e.tensor_tensor(out=sc["thr"], in0=sc["mid"], in1=sc["steps"][:, NIT:NIT + 1],
                                                      op=ALU.subtract),
                     reads=["mid" + t, "steps" + t], writes=["thr" + t])
            S.op("dve", lambda e: e.tensor_scalar(
                out=mtok[:, 0:ncol], in0=a[:, 0:ncol], scalar1=sc["thr"], scalar2=MASKV, op0=ALU.is_lt, op1=ALU.mult),
                reads=[akey, "thr" + t], writes=["mtok"])
            for g0 in range(0, tb + 1, 4):
                pb = 6 + psr % 2
                psr += 1
                blk = list(range(g0, min(g0 + 4, tb + 1)))
                for qi_, i in enumerate(blk):
                    S.op("pe", lambda e, qi_=qi_, i=i, pb=pb: e.transpose(
                        out=PS[pb][:, qi_ * 64:(qi_ + 1) * 64].bitcast(BF16), in_=mtok[:, i * 128:(i + 1) * 128],
                        identity=ident_bf),
                        reads=["mtok", "cbf"], writes=[("ps", pb)])
                for qi_, i in enumerate(blk):
                    mo = mtoff(i) + (tb - i) * 128
                    S.op("act", lambda e, qi_=qi_, mo=mo, pb=pb: e.activation(
                        out=maskv[:, mo:mo + 128], in_=PS[pb][:, qi_ * 64:(qi_ + 1) * 64].bitcast(BF16), func=AF.Identity),
                        reads=[("ps", pb)], writes=[("maskT", i)])

        for pi in range(8):
            items = []
            for q_ in range(2):
                tb = 2 * pi + q_
                ai = (2 * pi + q_) % 4
                items.append((tb, accs[ai], ("acc", ai), scr[q_]))
            if pi == 0:
                build_dg(0)
                build_dg(1)
            for it in items:
                do_scores(it[0], it[1], it[2])
            if pi < 7:
                build_dg(2 * pi + 2)
                build_dg(2 * pi + 3)
            for it in items:
                prep(*it)
            if pi >= 1:
                for k in range(NIT):
                    for fnk in (it_pass, it_u, it_mid):
                        for it in items:
                            fnk(*it, k)
            for it in items:
                fin(*it)
        S.barrier()
        if stop_after == "D":
            return finish(nc, S, es, dbg, dbg_d, locals())

        S.op("pool", lambda e: e.memset(bmT, 0.0), writes=["bmT"])
        QB = {0: (0, 1024, 2048, 3072), 1: (5200, 6224, 7248, 8272)}
        mbt = small[:, 560:624].bitcast(BF16)

        def load_unit(u):
            grp, h = u // 8, u % 8
            qb, kb, vb, zb = QB[grp]
            load_slab(qb + 128 * h, 128, [(wq, 0, 128, "wq")])
            load_slab(kb + 128 * h, 128, [(wk, 0, 128, "wk")])
            load_slab(vb + 128 * h, 128, [(wv, 0, 128, "wv")])
            load_slab(zb + 128 * h, 128, [(wz, 0, 128, "wz")])

        load_unit(UNITS[0])
        for ui, u in enumerate(UNITS):
            grp, h = u // 8, u % 8
            slope = 2.0 ** (-(h + 1))
            kcol = 1 if grp == 0 else 4
            proj_norm_pipe([(wq, "wq", tc, gq[:, u:u + 1], qT[:, tc * 512:(tc + 1) * 512], ("qT", tc)) for tc in range(4)] +
                           [(wk, "wk", tc, gains[:, kcol:kcol + 1], kT[:, tc * 512:(tc + 1) * 512], ("kT", tc))
                            for tc in range(4)])
            if stop_after == "E1" and ui == STOP_UI:
                return finish(nc, S, es, dbg, dbg_d, locals())
            if grp == 1:
                S.op("dve", lambda e: e.tensor_reduce(out=ksum, in_=kT.rearrange("p (n s) -> p n s", n=8), axis=AX.X,
                                                      op=ALU.add),
                     reads=[("kT", i) for i in range(4)], writes=["ksum"])
                S.op("dve", lambda e: e.tensor_copy(out=ksb, in_=ksum), reads=["ksum"], writes=["ksb"])
                for tb in range(NB):
                    S.op("pe", lambda e, tb=tb: e.matmul(out=PS[7][:, tb * 8:(tb + 1) * 8], lhsT=qT[:, tb * 128:(tb + 1) * 128],
                                                         rhs=ksb, start=True, stop=True),
                         reads=[("qT", tb // 4), "ksb"], writes=[("ps", 7)])
                for tb in range(NB):
                    n = tb // 2
                    S.op("dve", lambda e, tb=tb: e.tensor_copy(out=gt, in_=PS[7][:, tb * 8:(tb + 1) * 8]),
                         reads=[("ps", 7)], writes=["gt"])
                    if n < 8:
                        S.op("dve", lambda e, n=n: e.memset(gt[:, n:8], NEG), reads=["gt"], writes=["gt"])
                    S.op("dve", lambda e: e.max(out=m8, in_=gt), reads=["gt"], writes=["m8"])
                    S.op("dve", lambda e, tb=tb: e.tensor_scalar(out=mbt[:, tb * 8:(tb + 1) * 8], in0=gt, scalar1=m8[:, 2:3],
                                                                 scalar2=MASKV, op0=ALU.is_lt, op1=ALU.mult),
                         reads=["gt", "m8"], writes=["mbt"])
                for g in range(2):
                    pb = next_ps(2, 0)
                    for q_ in range(8):
                        tb = 8 * g + q_
                        S.op("pe", lambda e, tb=tb, q_=q_, pb=pb: e.transpose(
                            out=PS[pb][0:8, q_ * 64:(q_ + 1) * 64].bitcast(BF16), in_=mbt[:, tb * 8:(tb + 1) * 8],
                            identity=ident_bf),
                            reads=["mbt", "cbf"], writes=[("ps", pb)])
                    S.op("act", lambda e, g=g, pb=pb: e.activation(
                        out=bmT[0:8, g * 1024:(g + 1) * 1024], in_=PS[pb][0:8, :].bitcast(BF16), func=AF.Identity),
                        reads=[("ps", pb)], writes=["bmT"])
            for g in range(4):
                pb = next_ps(2, 0)
                for q_ in range(4):
                    tb = 4 * g + q_
                    for dh in range(16):
                        S.op("pe", lambda e, tb=tb, dh=dh, pb=pb, q_=q_: e.matmul(
                            out=PS[pb][:, q_ * 128:(q_ + 1) * 128], lhsT=hT[:, dh, tb * 128:(tb + 1) * 128],
                            rhs=wv[:, dh, :], start=(dh == 0), stop=(dh == 15)),
                            reads=["wv", ("hT", tb)], writes=[("ps", pb)])
                S.op("dve", lambda e, g=g, pb=pb: e.tensor_copy(
                    out=RP[:, 6144 + g * 512:6144 + (g + 1) * 512], in_=PS[pb][:, :]),
                    reads=[("ps", pb)], writes=[("V", g)])
            for tc in range(4):
                pb = next_ps(2, 0)
                proj_fm(wz, "wz", tc, pb)
                S.op("act", lambda e, tc=tc, pb=pb: e.activation(out=szT[:, tc * 512:(tc + 1) * 512], in_=PS[pb][:, :],
                                                                 func=AF.Silu),
                     reads=[("ps", pb)], writes=[("szT", tc)])
            if stop_after == "E2" and ui == STOP_UI:
                return finish(nc, S, es, dbg, dbg_d, locals())
            if ui + 1 < len(UNITS):
                load_unit(UNITS[ui + 1])
            pslot = 0
            sbank = 0
            for j in range(4):
                tb0 = 4 * j
                o1 = 4 + 2 * (j % 2)
                o2 = 5 + 2 * (j % 2)
                ni = 4 * j + 4
                def emit_S(i):
                    nonlocal_state = state
                    tlo = max(tb0, i)
                    c0 = (tlo - tb0) * 128
                    sb_ = state["sbank"] % 4
                    state["sbank"] += 1
                    nprime = i // 2
                    mm = []
                    mm.append((kT[:, i * 128:(i + 1) * 128], qT[:, tlo * 128:(tb0 + 4) * 128], c0, 512,
                               [("kT", i // 4), ("qT", j)]))
                    mm.append((atab, btab[:, c0:512], c0, 512, ["cbf"]))
                    if grp == 0:
                        mo = mtoff(i) + (tlo - i) * 128
                        mm.append((ident_bf, maskv[:, mo:mo + 512 - c0], c0, 512, [("maskT", i), "cbf"]))
                    else:
                        gl = max(tlo, 2 * nprime + 2)
                        if gl < tb0 + 4:
                            gc = (gl - tb0) * 128
                            mm.append((ident_bf[:, nprime:nprime + 1].to_broadcast([128, 128]),
                                       bmT[:, gl * 128:(tb0 + 4) * 128], gc, 512, ["bmT", "cbf"]))
                        if i >= tb0:
                            dc_ = (i - tb0) * 128
                            mm.append((ident_bf, ctri, dc_, dc_ + 128, ["cbf"]))
                    for mi, (lh, rh, a_, b_, rk) in enumerate(mm):
                        S.op("pe", lambda e, lh=lh, rh=rh, a_=a_, b_=b_, sb_=sb_, mi=mi, last=(mi == len(mm) - 1): e.matmul(
                            out=PS[sb_][:, a_:b_], lhsT=lh, rhs=rh, start=(mi == 0), stop=last),
                            reads=rk, writes=[("ps", sb_)])
                    return sb_, c0

                def emit_exp(i, sb_, c0):
                    ps_ = state["pslot"] % 4
                    state["pslot"] += 1
                    bcol = h * 19 + (i - tb0 + 15)
                    S.op("act", lambda e, bcol=bcol, slope=slope: e.activation(
                        out=Pt[ps_][:, c0:512], in_=PS[sb_][:, c0:512], func=AF.Exp,
                        bias=biasc[:, bcol:bcol + 1], scale=slope),
                        reads=[("ps", sb_), "biasc"], writes=[("P", ps_)])
                    return ps_

                def emit_PV(i, ps_, c0):
                    S.op("pe", lambda e, o1=o1, ni=ni: e.matmul(
                        out=PS[o1][:, c0:512], lhsT=Vv[:, i, :], rhs=Pt[ps_][:, c0:512], start=(i == 0), stop=(i == ni - 1)),
                        reads=[("P", ps_), ("V", i // 4)], writes=[("ps", o1)])
                    S.op("pe", lambda e, o2=o2, ni=ni: e.matmul(
                        out=PS[o2][:, c0:512], lhsT=ones_bf, rhs=Pt[ps_][:, c0:512], start=(i == 0), stop=(i == ni - 1)),
                        reads=[("P", ps_), "cbf"], writes=[("ps", o2)])

                LA = 2
                info = {}
                for i in range(min(LA, ni)):
                    info[i] = emit_S(i)
                for i in range(ni):
                    if i + LA < ni:
                        info[i + LA] = emit_S(i + LA)
                    ps_ = emit_exp(i, *info[i])
                    emit_PV(i, ps_, info[i][1])
                if stop_after == "E3" and ui == STOP_UI:
                    return finish(nc, S, es, dbg, dbg_d, locals())
                S.op("act", lambda e, o2=o2: e.activation(out=sd, in_=PS[o2][:, :], func=AF.Ln), reads=[("ps", o2)],
                     writes=["sd"])
                S.op("act", lambda e: e.activation(out=sd, in_=sd, func=AF.Exp, scale=-1.0), reads=["sd"], writes=["sd"])
                S.op("dve", lambda e, j=j: e.tensor_tensor(out=sd, in0=sd, in1=szT[:, j * 512:(j + 1) * 512], op=ALU.mult),
                     reads=["sd", ("szT", j)], writes=["sd"])
                S.op("dve", lambda e, u=u, j=j, o1=o1: e.tensor_tensor(
                    out=yT[:, u, j * 512:(j + 1) * 512], in0=PS[o1][:, :], in1=sd, op=ALU.mult),
                    reads=[("ps", o1), "sd"], writes=[("yT", 4 * j), ("yT", 4 * j + 1), ("yT", 4 * j + 2), ("yT", 4 * j + 3)])
            if stop_after == "E4" and ui == STOP_UI:
                return finish(nc, S, es, dbg, dbg_d, locals())
            if u == 7:
                S.barrier()
        S.barrier()
        if stop_after == "E":
            return finish(nc, S, es, dbg, dbg_d, locals())

        wout_v = wout_d.rearrange("(mh ml) n -> ml mh n", ml=128)
        wob = hT
        for cc in range(16):
            sl = state["stg"] % 2
            state["stg"] += 1
            c0 = cc * 128
            S.dma(lambda e, sl=sl, c0=c0: e.dma_start(out=stg[sl], in_=wout_v[:, :, c0:c0 + 128]), writes=[("stg", sl)])
            S.op("act", lambda e, sl=sl, c0=c0: e.activation(out=wob[:, :, c0:c0 + 128], in_=stg[sl], func=AF.Identity),
                 reads=[("stg", sl)], writes=[("wob", cc // 4)])
        xo = xt[0]
        ot = xt[1]
        out_toks = []
        for tb in range(NB):
            S.dma(lambda e, tb=tb: e.dma_start(out=xo, in_=x_d[tb * 128:(tb + 1) * 128, :]), writes=["xo"])
            for dc in range(4):
                pb = next_ps(4, 0)
                for mh in range(16):
                    S.op("pe", lambda e, tb=tb, dc=dc, mh=mh, pb=pb: e.matmul(
                        out=PS[pb][:, :], lhsT=yT[:, mh, tb * 128:(tb + 1) * 128], rhs=wob[:, mh, dc * 512:(dc + 1) * 512],
                        start=(mh == 0), stop=(mh == 15)),
                        reads=[("yT", tb), ("wob", dc)], writes=[("ps", pb)])
                S.op("dve", lambda e, dc=dc, pb=pb: e.tensor_tensor(
                    out=ot[:, dc * 512:(dc + 1) * 512], in0=PS[pb][:, :], in1=gatebc[:, dc * 512:(dc + 1) * 512], op=ALU.mult),
                    reads=[("ps", pb), "gatebc"], writes=["ot"])
                S.op("dve", lambda e, dc=dc: e.tensor_tensor(
                    out=ot[:, dc * 512:(dc + 1) * 512], in0=ot[:, dc * 512:(dc + 1) * 512],
                    in1=xo[:, dc * 512:(dc + 1) * 512], op=ALU.add),
                    reads=["ot", "xo"], writes=["ot"])
            out_toks.append(S.dma(lambda e, tb=tb: e.dma_start(out=out_d[tb * 128:(tb + 1) * 128, :], in_=ot),
                                  reads=["ot"], writes=[("outd", tb)]))
        S.ops["sp"].append((None, out_toks, None))
        return finish(nc, S, es, None, None, locals())


def finish(nc, S, es, dbg, dbg_d, env):
    if dbg is not None:
        src = env[dbg[0]]
        S.barrier()
        t = S.dma(lambda e: e.dma_start(out=dbg_d, in_=dbg[3](env)), reads=[], writes=["dbgout"])
        S.ops["sp"].append((None, [t], None))
    from contextlib import ExitStack
    with ExitStack() as es2:
        sems = {e: es2.enter_context(nc.semaphore("sem_" + e)) for e in ENGS if e != "sp"}
        dsems = [es2.enter_context(nc.semaphore("dsem%d" % i)) for i in range(NDMASEM)]
        block = es2.enter_context(nc.Block())
        S.emit(nc, block, sems, dsems)
    return nc


def _consts():
    identf = np.eye(128, dtype=np.float32)
    pow2 = np.tile((2.0 ** -(np.arange(32) + 1.0)).astype(np.float32)[None, :], (128, 1))
    cb = np.zeros((128, 1664), dtype=np.float32)
    cb[:, 0:128] = np.eye(128)
    cb[:, 128:256] = 1.0
    bo = np.zeros((128, 128), np.float32)
    bo[:64, :64] = 1.0
    bo[64:, 64:] = 1.0
    cb[:, 256:384] = bo
    sidx = np.arange(128)[:, None]
    tidx = np.arange(128)[None, :]
    cb[:, 384:512] = np.where(sidx > tidx, MASKV, 0.0)
    at = np.zeros((128, 128), np.float32)
    at[0, :] = np.arange(128)
    at[1, :] = 1.0
    at[2, :] = 1.0
    cb[:, 512:640] = at
    bt = np.zeros((128, 512), np.float32)
    tr = np.arange(512)
    bt[0, :] = 1.0
    bt[1, :] = -128.0 * (tr // 128)
    bt[2, :] = -(tr % 128)
    cb[:, 640:1152] = bt
    bc = np.zeros((128, 152), np.float32)
    for h in range(8):
        for v in range(-15, 4):
            bc[:, h * 19 + v + 15] = (2.0 ** -(h + 1)) * 128.0 * v
    return identf, pow2, cb.astype(ml_dtypes.bfloat16), bc


def _in_maps(inputs):
    x = np.asarray(inputs["x"], np.float32)
    c = np.asarray(inputs["c"], np.float32)
    w_ada = np.ascontiguousarray(np.asarray(inputs["w_ada"], np.float32)[0])
    b_ada = np.asarray(inputs["b_ada"], np.float32)[0]
    g_norm = np.asarray(inputs["g_norm"], np.float32)[0]
    w_in = np.ascontiguousarray(np.asarray(inputs["w_in"], np.float32)[0])
    w_out = np.ascontiguousarray(np.asarray(inputs["w_out"], np.float32)[0])
    lay = lambda v: np.ascontiguousarray(v.reshape(16, 128).T)
    kni = np.asarray(inputs["k_norm_idx"], np.float32)[0]
    gains = np.stack([np.asarray(inputs["q_norm_a"], np.float32)[0], np.asarray(inputs["k_norm_a"], np.float32)[0],
                      np.concatenate([kni, kni]), np.asarray(inputs["q_norm_b"], np.float32)[0],
                      np.asarray(inputs["k_norm_b"], np.float32)[0]], axis=1).astype(np.float32)
    identf, pow2, cbf, biasc = _consts()
    maps = []
    for b in range(8):
        maps.append({
            "x": np.ascontiguousarray(x[b]), "c2": lay(c[b]), "w_ada": w_ada,
            "bsh": lay(b_ada[0:D]), "bsc": lay(b_ada[D:2 * D]), "bgate": np.ascontiguousarray(b_ada[2 * D:3 * D][None, :]),
            "g2": lay(g_norm), "w_in": w_in, "gains": np.ascontiguousarray(gains), "w_out": w_out,
            "identf": identf, "pow2": pow2, "cbf": cbf, "biasc": biasc,
        })
    return maps


_NC_CACHE = {}


def kernel(**inputs):
    if "nc" not in _NC_CACHE:
        _NC_CACHE["nc"] = build_nc()
    nc = _NC_CACHE["nc"]
    maps = _in_maps(inputs)
    res = run_bass_kernel_spmd(nc, maps, core_ids=list(range(8)))
    out = np.stack([np.asarray(r["out"], np.float32) for r in res.results], axis=0)
    return out
```
